# Optimizing a Trainium2 kernel written in Bass

```python
import jax, jax.numpy as jnp
from jax import lax
import numpy as np

D_MODEL = 1024
BATCH = 4
SEQ = 4096
DEPTH = 2

GRID_W = 64
CTX_LEN = 256
MIX_WIDTH = D_MODEL // 2
HEAD_DIM = 64
GQA_HEADS = MIX_WIDTH // HEAD_DIM
GQA_KV_HEADS = GQA_HEADS // 4
NA_HEADS = MIX_WIDTH // HEAD_DIM
NA_WIN_ROWS = 8
NA_WIN_COLS = 16
FOURIER_WIDTH = MIX_WIDTH
FOURIER_GROUPS = 4
FOURIER_GROUP_DIM = FOURIER_WIDTH // FOURIER_GROUPS
CONV_WIDTH = MIX_WIDTH
CONV_KERNEL = 31
N_BRANCHES = 4
N_EXPERTS = 16
N_EXPERT_GROUPS = 4
EXPERTS_PER_GROUP = N_EXPERTS // N_EXPERT_GROUPS
TOP_K = 2
EXPERT_FF = D_MODEL // 2
Q_BLOCK = 128
ROPE_THETA = 10000.0
EPS = 1e-6
ATTN_SCALE = HEAD_DIM ** -0.5

GQA_Q_WIDTH = GQA_HEADS * HEAD_DIM
GQA_KV_WIDTH = GQA_KV_HEADS * HEAD_DIM
NA_WIDTH = NA_HEADS * HEAD_DIM
PROJ_SIZES = (GQA_Q_WIDTH, GQA_KV_WIDTH, GQA_KV_WIDTH, NA_WIDTH, NA_WIDTH, NA_WIDTH,
              FOURIER_WIDTH, 2 * CONV_WIDTH, N_BRANCHES * D_MODEL)
PROJ_SPLITS = tuple(int(v) for v in np.cumsum(PROJ_SIZES)[:-1])
IN_WIDTH = sum(PROJ_SIZES)

kernel_name = "hybrid_parallel_mixer_moe_dit"


def rms_norm(x, g):
    xf = x.astype(jnp.float32)
    y = xf * lax.rsqrt(jnp.mean(xf * xf, axis=-1, keepdims=True) + EPS)
    return (y * g.astype(jnp.float32)).astype(x.dtype)


def axial_rope(n_tokens):
    t = jnp.arange(n_tokens, dtype=jnp.int32)
    row = (t // GRID_W).astype(jnp.float32)
    col = (t % GRID_W).astype(jnp.float32)
    n_pairs = HEAD_DIM // 4
    inv_freq = ROPE_THETA ** (-jnp.arange(n_pairs, dtype=jnp.float32) / n_pairs)
    ang = jnp.concatenate([row[:, None] * inv_freq, col[:, None] * inv_freq], axis=-1)
    return jnp.cos(ang), jnp.sin(ang)


def apply_rope(x, cos, sin):
    xf = x.astype(jnp.float32).reshape(*x.shape[:-1], HEAD_DIM // 2, 2)
    x1, x2 = xf[..., 0], xf[..., 1]
    c = cos[None, :, None, :]
    s = sin[None, :, None, :]
    return jnp.stack([x1 * c - x2 * s, x1 * s + x2 * c], axis=-1).reshape(x.shape).astype(x.dtype)


def attend(qg, k, v):
    s = jnp.einsum('bqhgd,bkhd->bhgqk', qg, k, preferred_element_type=jnp.float32) * ATTN_SCALE
    p = jax.nn.softmax(s, axis=-1).astype(v.dtype)
    return jnp.einsum('bhgqk,bkhd->bqhgd', p, v)


def dense_attention(q, k, v):
    B, L, hq, _ = q.shape
    hkv = k.shape[2]
    out = attend(q.reshape(B, L, hkv, hq // hkv, HEAD_DIM), k, v)
    return out.reshape(B, L, hq * HEAD_DIM)


def gqa_latent(q, k, v, k_ctx, v_ctx):
    B, S = q.shape[:2]
    g = GQA_HEADS // GQA_KV_HEADS
    k_all = jnp.concatenate([k, k_ctx], axis=1)
    v_all = jnp.concatenate([v, v_ctx], axis=1)
    nb = S // Q_BLOCK
    qb = q.reshape(B, nb, Q_BLOCK, GQA_KV_HEADS, g, HEAD_DIM).transpose(1, 0, 2, 3, 4, 5)
    out = lax.map(lambda qblk: attend(qblk, k_all, v_all), qb)
    return out.transpose(1, 0, 2, 3, 4, 5).reshape(B, S, GQA_Q_WIDTH)


def neighbourhood_latent(q, k, v, k_ctx, v_ctx, rpb):
    B, S = q.shape[:2]
    rows = S // GRID_W
    wr = min(NA_WIN_ROWS, rows)
    qg = q.reshape(B, rows, GRID_W, NA_HEADS, HEAD_DIM)
    kg = k.reshape(B, rows, GRID_W, NA_HEADS, HEAD_DIM)
    vg = v.reshape(B, rows, GRID_W, NA_HEADS, HEAD_DIM)
    cols = np.arange(GRID_W)
    col_start = np.clip(cols - NA_WIN_COLS // 2, 0, GRID_W - NA_WIN_COLS)
    col_idx = col_start[:, None] + np.arange(NA_WIN_COLS)[None, :]
    col_bias_idx = col_idx - cols[:, None] + (NA_WIN_COLS - 1)
    row_starts = jnp.clip(jnp.arange(rows) - NA_WIN_ROWS // 2, 0, rows - wr)
    rpb_f = rpb.astype(jnp.float32)
    n_win = wr * NA_WIN_COLS

    def row_block(args):
        r, r0, q_row = args
        k_band = lax.dynamic_slice_in_dim(kg, r0, wr, axis=1)
        v_band = lax.dynamic_slice_in_dim(vg, r0, wr, axis=1)
        k_win = k_band[:, :, col_idx]
        v_win = v_band[:, :, col_idx]
        s_win = jnp.einsum('bqhd,bwqjhd->bhqwj', q_row, k_win,
                           preferred_element_type=jnp.float32) * ATTN_SCALE
        row_bias_idx = r0 + jnp.arange(wr) - r + (NA_WIN_ROWS - 1)
        bias = rpb_f[:, row_bias_idx][:, :, col_bias_idx]
        s_win = s_win + bias.transpose(0, 2, 1, 3)[None]
        s_ctx = jnp.einsum('bqhd,bkhd->bhqk', q_row, k_ctx,
                           preferred_element_type=jnp.float32) * ATTN_SCALE
        s = jnp.concatenate([s_win.reshape(B, NA_HEADS, GRID_W, n_win), s_ctx], axis=-1)
        p = jax.nn.softmax(s, axis=-1).astype(v.dtype)
        p_win = p[..., :n_win].reshape(B, NA_HEADS, GRID_W, wr, NA_WIN_COLS)
        p_ctx = p[..., n_win:]
        return (jnp.einsum('bhqwj,bwqjhd->bqhd', p_win, v_win)
                + jnp.einsum('bhqk,bkhd->bqhd', p_ctx, v_ctx))

    out = lax.map(row_block, (jnp.arange(rows), row_starts, qg.transpose(1, 0, 2, 3, 4)))
    return out.transpose(1, 0, 2, 3, 4).reshape(B, S, NA_WIDTH)


def fourier_mix(u):
    B, L = u.shape[:2]
    ug = u.astype(jnp.float32).reshape(B, L, FOURIER_GROUPS, FOURIER_GROUP_DIM)
    y = jnp.fft.fft2(ug, axes=(1, 3), norm='ortho').real
    return y.reshape(B, L, FOURIER_WIDTH).astype(u.dtype)


def conformer_conv(u, conv_w, conv_b, conv_g):
    a, b = jnp.split(u, 2, axis=-1)
    z = a * jax.nn.sigmoid(b)
    z = lax.conv_general_dilated(z, conv_w[:, None, :], window_strides=(1,),
                                 padding=[(CONV_KERNEL // 2, CONV_KERNEL // 2)],
                                 dimension_numbers=('NWC', 'WIO', 'NWC'),
                                 feature_group_count=CONV_WIDTH) + conv_b
    return jax.nn.silu(rms_norm(z, conv_g))


def merge_branches(branches, gate_logits, w_branch, w_out):
    y = jnp.stack(branches, axis=-2)
    z = jnp.einsum('blnc,ncd->blnd', y, w_branch)
    g = jax.nn.sigmoid(gate_logits.reshape(*gate_logits.shape[:-1], N_BRANCHES, D_MODEL)
                       .astype(jnp.float32)).astype(z.dtype)
    return jnp.sum(g * z, axis=-2) @ w_out


def token_mixer(h, hc, w_in, q_g, k_g, rpb, conv_w, conv_b, conv_g, w_branch, w_out, cos, sin, update_ctx):
    B, S, _ = h.shape
    C = hc.shape[1]
    gq, gk, gv, nq, nk, nv, fu, cu, gl = jnp.split(h @ w_in, PROJ_SPLITS, axis=-1)
    cgq, cgk, cgv, cnq, cnk, cnv, cfu, ccu, cgl = jnp.split(hc @ w_in, PROJ_SPLITS, axis=-1)
    q = apply_rope(rms_norm(gq.reshape(B, S, GQA_HEADS, HEAD_DIM), q_g), cos, sin)
    k = apply_rope(rms_norm(gk.reshape(B, S, GQA_KV_HEADS, HEAD_DIM), k_g), cos, sin)
    v = gv.reshape(B, S, GQA_KV_HEADS, HEAD_DIM)
    kc = rms_norm(cgk.reshape(B, C, GQA_KV_HEADS, HEAD_DIM), k_g)
    vc = cgv.reshape(B, C, GQA_KV_HEADS, HEAD_DIM)
    y_gqa = gqa_latent(q, k, v, kc, vc)
    nkc = cnk.reshape(B, C, NA_HEADS, HEAD_DIM)
    nvc = cnv.reshape(B, C, NA_HEADS, HEAD_DIM)
    y_na = neighbourhood_latent(nq.reshape(B, S, NA_HEADS, HEAD_DIM), nk.reshape(B, S, NA_HEADS, HEAD_DIM),
                                nv.reshape(B, S, NA_HEADS, HEAD_DIM), nkc, nvc, rpb)
    y_fn = fourier_mix(fu)
    y_cv = conformer_conv(cu, conv_w, conv_b, conv_g)
    out = merge_branches((y_gqa, y_na, y_fn, y_cv), gl, w_branch, w_out)
    if not update_ctx:
        return out, None
    qc = rms_norm(cgq.reshape(B, C, GQA_HEADS, HEAD_DIM), q_g)
    yc_gqa = dense_attention(qc, kc, vc)
    yc_na = dense_attention(cnq.reshape(B, C, NA_HEADS, HEAD_DIM), nkc, nvc)
    yc_fn = fourier_mix(cfu)
    yc_cv = conformer_conv(ccu, conv_w, conv_b, conv_g)
    out_c = merge_branches((yc_gqa, yc_na, yc_fn, yc_cv), cgl, w_branch, w_out)
    return out, out_c


def moe(h, w_router, router_bias, w_gu, w_down):
    lead = h.shape[:-1]
    t = h.reshape(-1, D_MODEL)
    scores = jax.nn.sigmoid((t @ w_router).astype(jnp.float32))
    sel = scores + router_bias.astype(jnp.float32)
    group_score = lax.top_k(sel.reshape(-1, N_EXPERT_GROUPS, EXPERTS_PER_GROUP), TOP_K)[0].sum(-1)
    g_idx = jnp.argmax(group_score, axis=-1)
    in_group = (jnp.arange(N_EXPERTS) // EXPERTS_PER_GROUP)[None, :] == g_idx[:, None]
    _, top_idx = lax.top_k(jnp.where(in_group, sel, -jnp.inf), TOP_K)
    w = jnp.take_along_axis(scores, top_idx, axis=-1)
    w = w / jnp.sum(w, axis=-1, keepdims=True)
    gate = jnp.sum(jax.nn.one_hot(top_idx, N_EXPERTS, dtype=jnp.float32) * w[..., None], axis=1).astype(t.dtype)
    out = jnp.zeros_like(t)
    for e in range(N_EXPERTS):
        a, b = jnp.split(t @ w_gu[e], 2, axis=-1)
        out = out + gate[:, e:e + 1] * ((jax.nn.silu(a) * b) @ w_down[e])
    return out.reshape(*lead, D_MODEL)


def setup_inputs(seed: int = 0) -> dict:
    key = jax.random.key(seed)
    ks = jax.random.split(key, 22)
    D = D_MODEL

    def nrm(k, shape, std):
        return jax.random.normal(k, shape, jnp.float32) * std

    return {
        "x": nrm(ks[0], (BATCH, SEQ, D), 1.0),
        "c": nrm(ks[1], (BATCH, D), 1.0),
        "ctx": nrm(ks[2], (BATCH, CTX_LEN, D), 1.0),
        "c_ctx": nrm(ks[3], (D,), 1.0),
        "w_mod": nrm(ks[4], (DEPTH, D, 6 * D), 0.5 * D ** -0.5),
        "b_mod": nrm(ks[5], (DEPTH, 6 * D), 0.02),
        "norm1_g": 1.0 + nrm(ks[6], (DEPTH, D), 0.02),
        "norm2_g": 1.0 + nrm(ks[7], (DEPTH, D), 0.02),
        "w_in": nrm(ks[8], (DEPTH, D, IN_WIDTH), D ** -0.5),
        "q_norm_g": 1.0 + nrm(ks[9], (DEPTH, HEAD_DIM), 0.02),
        "k_norm_g": 1.0 + nrm(ks[10], (DEPTH, HEAD_DIM), 0.02),
        "na_rpb": nrm(ks[11], (DEPTH, NA_HEADS, 2 * NA_WIN_ROWS - 1, 2 * NA_WIN_COLS - 1), 0.02),
        "conv_w": nrm(ks[12], (DEPTH, CONV_KERNEL, CONV_WIDTH), CONV_KERNEL ** -0.5),
        "conv_b": nrm(ks[13], (DEPTH, CONV_WIDTH), 0.01),
        "conv_norm_g": 1.0 + nrm(ks[14], (DEPTH, CONV_WIDTH), 0.02),
        "w_branch": nrm(ks[15], (DEPTH, N_BRANCHES, MIX_WIDTH, D), MIX_WIDTH ** -0.5),
        "w_out": nrm(ks[16], (DEPTH, D, D), D ** -0.5),
        "w_router": nrm(ks[17], (D, N_EXPERTS), D ** -0.5),
        "router_bias": nrm(ks[18], (N_EXPERTS,), 0.01),
        "w_expert_gu": nrm(ks[19], (DEPTH, N_EXPERTS, D, 2 * EXPERT_FF), D ** -0.5),
        "w_expert_down": nrm(ks[20], (DEPTH, N_EXPERTS, EXPERT_FF, D), EXPERT_FF ** -0.5),
        "final_norm_g": 1.0 + nrm(ks[21], (D,), 0.02),
    }


def reference(x, c, ctx, c_ctx, w_mod, b_mod, norm1_g, norm2_g, w_in, q_norm_g, k_norm_g, na_rpb,
              conv_w, conv_b, conv_norm_g, w_branch, w_out, w_router, router_bias,
              w_expert_gu, w_expert_down, final_norm_g):
    S = x.shape[1]
    cos, sin = axial_rope(S)
    xc = ctx
    for l in range(DEPTH):
        update_ctx = l < DEPTH - 1
        m = jax.nn.silu(c) @ w_mod[l] + b_mod[l]
        sh1, sc1, g1, sh2, sc2, g2 = [t[:, None, :] for t in jnp.split(m, 6, axis=-1)]
        mc = jax.nn.silu(c_ctx) @ w_mod[l] + b_mod[l]
        csh1, csc1, cg1, csh2, csc2, cg2 = jnp.split(mc, 6, axis=-1)
        h = rms_norm(x, norm1_g[l]) * (1.0 + sc1) + sh1
        hc = rms_norm(xc, norm1_g[l]) * (1.0 + csc1) + csh1
        y, yc = token_mixer(h, hc, w_in[l], q_norm_g[l], k_norm_g[l], na_rpb[l], conv_w[l], conv_b[l],
                            conv_norm_g[l], w_branch[l], w_out[l], cos, sin, update_ctx)
        x = x + g1 * y
        h2 = rms_norm(x, norm2_g[l]) * (1.0 + sc2) + sh2
        if update_ctx:
            xc = xc + cg1 * yc
            h2c = rms_norm(xc, norm2_g[l]) * (1.0 + csc2) + csh2
            z = moe(jnp.concatenate([h2, h2c], axis=1), w_router, router_bias, w_expert_gu[l], w_expert_down[l])
            x = x + g2 * z[:, :S]
            xc = xc + cg2 * z[:, S:]
        else:
            x = x + g2 * moe(h2, w_router, router_bias, w_expert_gu[l], w_expert_down[l])
    return rms_norm(x, final_norm_g)
```

```python
import contextlib
import numpy as np
import concourse.bass as bass
import concourse.mybir as mybir
from concourse.bass_utils import run_bass_kernel_spmd

F32 = mybir.dt.float32
BF16 = mybir.dt.bfloat16
AF = mybir.ActivationFunctionType
ALU = mybir.AluOpType
AX = mybir.AxisListType

D = 1024
S = 4096
C = 256
T = S + C
NT = T // 128
DEPTH = 2
INW = 7936
EPS = 1e-6
NE = 16
BIG = 1.0e9

DEBUG = False
STOP_AFTER = None
NLAYERS = DEPTH


class Tile:
    __slots__ = ("ap", "lw", "rd", "name")

    def __init__(self, ap, name=""):
        self.ap = ap
        self.lw = None
        self.rd = {}
        self.name = name

    def __getitem__(self, idx):
        return self.ap[idx]


class Sched:
    def __init__(self, nc, st):
        self.nc = nc
        self.eng = {"pe": nc.tensor, "act": nc.scalar, "dve": nc.vector, "pool": nc.gpsimd, "sp": nc.sync}
        self.st = st
        self.epoch = 0
        self.semobjs = {}
        self.sem = {}
        self.ekey = {}
        self.cnt = {}
        self.seen = {k: {} for k in self.eng}
        self._new_epoch()
        self.ND = 40
        self.dsem = [st.enter_context(nc.semaphore("dsem%d" % i)) for i in range(self.ND)]
        self.dcnt = [0] * self.ND
        self.dnext = 0
        self.NDH = 28
        self.dnext_sw = 0

    def _new_epoch(self):
        self.epoch += 1
        for k in self.eng:
            key = (k, self.epoch)
            self.semobjs[key] = self.st.enter_context(self.nc.semaphore("sem_%s_%d" % (k, self.epoch)))
            self.sem[k] = self.semobjs[key]
            self.ekey[k] = key
            self.cnt[k] = 0

    def _semobj(self, key):
        return self.dsem[key[1]] if key[0] == "d" else self.semobjs[key]

    def _wait(self, e, deps):
        best = {}
        for key, v in deps:
            if key[0] == "pe" and e == "pe":
                continue
            if v > best.get(key, 0):
                best[key] = v
        for key, v in best.items():
            if self.seen[e].get(key, 0) >= v:
                continue
            self.eng[e].wait_ge(self._semobj(key), v)
            self.seen[e][key] = v

    @staticmethod
    def _deps(r, w):
        deps = []
        for t in r:
            if t.lw is not None:
                deps.append(t.lw)
        for t in w:
            if t.lw is not None:
                deps.append(t.lw)
            deps.extend(t.rd.items())
        return deps

    @staticmethod
    def _stamp(r, w, key, v):
        for t in w:
            t.lw = (key, v)
            t.rd = {}
        for t in r:
            if t.rd.get(key, 0) < v:
                t.rd[key] = v

    def op(self, e, fn, r=(), w=()):
        self._wait(e, self._deps(r, w))
        ins = fn(self.eng[e])
        self.cnt[e] += 1
        ins.then_inc(self.sem[e], 1)
        self._stamp(r, w, self.ekey[e], self.cnt[e])

    def dma(self, q, out, in_, r=(), w=()):
        if q == "pool":
            i = self.NDH + self.dnext_sw
            self.dnext_sw = (self.dnext_sw + 1) % (self.ND - self.NDH)
        else:
            i = self.dnext
            self.dnext = (i + 1) % self.NDH
        deps = self._deps(r, w)
        if self.dcnt[i] > 0:
            deps.append((("d", i), self.dcnt[i]))
        self._wait(q, deps)
        self.eng[q].dma_start(out=out, in_=in_).then_inc(self.dsem[i], 16)
        self.dcnt[i] += 16
        self._stamp(r, w, ("d", i), self.dcnt[i])

    def barrier(self, new_epoch=False):
        deps = [(self.ekey[k], self.cnt[k]) for k in self.eng if self.cnt[k] > 0]
        deps += [(("d", i), self.dcnt[i]) for i in range(self.ND) if self.dcnt[i] > 0]
        for e in self.eng:
            for key, v in deps:
                if self.seen[e].get(key, 0) >= v:
                    continue
                self.eng[e].wait_ge(self._semobj(key), v)
                self.seen[e][key] = v
        if new_epoch:
            self._new_epoch()


class Ring:
    def __init__(self, tiles):
        self.tiles = tiles
        self.i = 0

    def next(self):
        t = self.tiles[self.i % len(self.tiles)]
        self.i += 1
        return t


def build(nlayers=NLAYERS):
    nc = bass.Bass("TRN2", target_bir_lowering=False)
    st = contextlib.ExitStack()
    with st:
        _build(nc, st, nlayers)
    return nc


def _build(nc, st, nlayers):
    sc = Sched(nc, st)
    op, dma = sc.op, sc.dma

    def din(name, shape):
        return nc.dram_tensor(name, list(shape), F32, kind="ExternalInput").ap()

    SK = "ExternalOutput" if DEBUG else "Internal"

    def dscr(name, shape, dt):
        return Tile(nc.dram_tensor(name, list(shape), dt, kind=SK).ap(), name)

    x_in = din("x", [S, D])
    ctx_in = din("ctx", [C, D])
    cvec_in = din("cvec", [128, 8, 2])
    w_mod = din("w_mod", [DEPTH, D, 6 * D])
    bmodT = din("bmodT", [DEPTH, 128, 48])
    n1g = din("n1g", [DEPTH, 128, 8])
    n2g = din("n2g", [DEPTH, 128, 8])
    w_in = din("w_in", [DEPTH, D, INW])
    qgc = din("qgc", [DEPTH, 128, 1])
    kgc = din("kgc", [DEPTH, 128, 1])
    rpbP = din("rpbP", [DEPTH, 8, 17, 127])
    cwT = din("cwT", [DEPTH, 128, 4, 31])
    cbT = din("cbT", [DEPTH, 128, 4])
    cgT = din("cgT", [DEPTH, 128, 4])
    w_branch = din("w_branch", [DEPTH, 4, 512, D])
    w_out = din("w_out", [DEPTH, D, D])
    wr_in = din("wr", [128, 8, NE])
    rb_in = din("rb", [128, 12, NE])
    SMALL_E = DEBUG and STOP_AFTER is not None and STOP_AFTER[1] != "E"
    w_gu = din("w_gu", [DEPTH, NE, D, D] if not SMALL_E else [1, 1, 128, 128])
    w_dn = din("w_dn", [DEPTH, NE, 512, D] if not SMALL_E else [1, 1, 128, 128])
    fng = din("fng", [128, D])
    k_c128 = din("c128", [128, 6 * 128])
    k_rope = din("rope", [128, 2, S])
    k_namask = din("namask", [128, 16 * 64])
    k_dft256 = din("dft256", [128, 2, 2, 256])
    k_f1 = din("f1", [64, 128])
    k_m12 = din("m12", [64, 2, 64 * 128])
    sel_in = din("sel", [128, 2])
    out_d = nc.dram_tensor("out", [S // 2, D], F32, kind="ExternalOutput").ap()

    X = dscr("X", [T, D], F32)
    X2 = dscr("X2", [S // 2, D], F32)
    QG = dscr("QG", [512, T], BF16)
    KG = dscr("KG", [256, T], BF16)
    VG = dscr("VG", [T, 2 * 128], BF16)
    QN = dscr("QN", [512, T], BF16)
    KN = dscr("KN", [512, T], BF16)
    VN = dscr("VN", [T, 8 * 128], BF16)
    U = dscr("U", [T, 512], BF16)
    ZC = dscr("ZC", [512, T], BF16)
    SG = dscr("SG", [4096, T], BF16)
    Y = dscr("Y", [2048, T], BF16)
    XIN = Tile(None, "xin")

    uid = [0]

    def dbg(name, tile, shape, dt=F32):
        if not DEBUG:
            return
        d_ = nc.dram_tensor("dbg_" + name, list(shape), dt, kind="ExternalOutput").ap()
        dma("sp", d_, tile[:], r=[tile], w=[Tile(None)])

    def sb(name, shape, dt=BF16, stack=st):
        uid[0] += 1
        return Tile(stack.enter_context(nc.sbuf_tensor("s%d_%s" % (uid[0], name), list(shape), dt)), name)

    def ring(name, n, shape, dt=BF16, stack=st):
        return Ring([sb("%s%d" % (name, i), shape, dt, stack) for i in range(n)])

    PB = [Tile(st.enter_context(nc.psum_tensor("pb%d" % i, [128, 512], F32)), "pb%d" % i) for i in range(7)]
    ptn = [0]

    def alloc_pt(stack):
        ptn[0] += 1
        return Tile(stack.enter_context(nc.psum_tensor("pt%d" % ptn[0], [128, 1024], BF16)), "pt")

    def alloc_pb7(stack):
        ptn[0] += 1
        return Tile(stack.enter_context(nc.psum_tensor("pb7_%d" % ptn[0], [128, 512], F32)), "pb7")

    c128 = sb("c128", [128, 6, 128])
    identf = sb("identf", [128, 128], F32)
    onesf = sb("onesf", [128, 128], F32)
    cvec = sb("cvec", [128, 8, 2], F32)
    scv = sb("scv", [128, 8, 2])
    wr = sb("wr", [128, 8, NE])
    rbt = sb("rbt", [128, 12, NE], F32)
    fngt = sb("fngt", [128, D], F32)
    selt = sb("selt", [128, 2], F32)
    dma("pool", c128[:], k_c128.rearrange("p (a b) -> p a b", b=128), w=[c128])
    dma("sp", identf[:], k_c128[:, 0:128], w=[identf])
    dma("sp", onesf[:], k_c128[:, 128:256], w=[onesf])
    dma("sp", cvec[:], cvec_in, w=[cvec])
    dma("pool", wr[:], wr_in, w=[wr])
    dma("sp", rbt[:], rb_in, w=[rbt])
    dma("sp", fngt[:], fng, w=[fngt])
    dma("sp", selt[:], sel_in, w=[selt])

    def blend(ta, apa, tb, apb):
        op("dve", lambda e: e.tensor_scalar(out=apb, in0=apb, scalar1=selt[:, 1:2], scalar2=None, op0=ALU.mult),
           r=[tb, selt], w=[tb])
        op("dve", lambda e: e.scalar_tensor_tensor(out=apa, in0=apa, scalar=selt[:, 0:1], in1=apb, op0=ALU.mult, op1=ALU.add),
           r=[ta, tb, selt], w=[ta])
    op("act", lambda e: e.activation(out=scv[:], in_=cvec[:], func=AF.Silu), r=[cvec], w=[scv])
    ident = c128[:, 0, :]
    ones = c128[:, 1, :]
    bdones = c128[:, 2, :]
    Pm = c128[:, 3, :]
    CCm = c128[:, 4, :]
    SCm = c128[:, 5, :]

    mod = sb("mod", [128, 48, 2], F32)
    sc1 = sb("sc1", [128, 8, 2], F32)
    sc2 = sb("sc2", [128, 8, 2], F32)
    gbc = [[sb("gbc%d%d" % (a, b), [128, D], F32) for b in range(2)] for a in range(2)]
    bmt = sb("bmt", [128, 48], F32)
    n1t = sb("n1t", [128, 8], F32)
    n2t = sb("n2t", [128, 8], F32)
    qgt = sb("qgt", [128, 1], F32)
    kgt = sb("kgt", [128, 1], F32)
    cwt = sb("cwt", [128, 4, 31], F32)
    cbt = sb("cbt", [128, 4], F32)
    cgt = sb("cgt", [128, 4], F32)

    def xsrc(l, t):
        if l == 0:
            if t < 32:
                return x_in[t * 128:(t + 1) * 128, :], XIN
            return ctx_in[(t - 32) * 128:(t - 31) * 128, :], XIN
        return X[t * 128:(t + 1) * 128, :], X

    def done(l, ph):
        return STOP_AFTER is not None and (l, ph) == STOP_AFTER

    def norm_tile(ps, xt, hT, col0, scl, shf_chunk0, i, xn_ring, ss_ring, trk=None):
        trk = hT if trk is None else trk
        junk = xn_ring.next()
        ss = ss_ring.next()
        op("act", lambda e: e.activation(out=junk[:], in_=xt[:], func=AF.Square, accum_out=ss[:, 0:1]),
           r=[xt], w=[junk, ss])
        op("act", lambda e: e.activation(out=ss[:, 1:2], in_=ss[:, 0:1], func=AF.Sqrt, scale=1.0 / D, bias=epsc[:, 0:1]),
           r=[ss, epsc], w=[ss])
        op("dve", lambda e: e.reciprocal(out=ss[:, 2:3], in_=ss[:, 1:2]), r=[ss], w=[ss])
        xn = xn_ring.next()
        op("dve", lambda e: e.tensor_scalar(out=xn[:], in0=xt[:], scalar1=ss[:, 2:3], scalar2=None, op0=ALU.mult),
           r=[xt, ss], w=[xn])
        for k in range(8):
            op("pe", lambda e: e.transpose(out=ps[:, k * 128:(k + 1) * 128], in_=xn[:, k * 128:(k + 1) * 128], identity=ident),
               r=[xn, c128], w=[ps])
        for k in range(8):
            eng = "act" if k % 2 == 0 else "dve"
            if eng == "act":
                op("act", lambda e: e.activation(out=hT[:, k, col0:col0 + 128], in_=ps[:, k * 128:(k + 1) * 128],
                                                 func=AF.Identity, scale=scl[:, k, i:i + 1],
                                                 bias=mod[:, shf_chunk0 + k, i:i + 1]),
                   r=[ps, scl, mod], w=[trk])
            else:
                op("dve", lambda e: e.tensor_scalar(out=hT[:, k, col0:col0 + 128], in0=ps[:, k * 128:(k + 1) * 128],
                                                    scalar1=scl[:, k, i:i + 1], scalar2=mod[:, shf_chunk0 + k, i:i + 1],
                                                    op0=ALU.mult, op1=ALU.add),
                   r=[ps, scl, mod], w=[trk])

    epsc = sb("epsc", [128, 1], F32)
    op("dve", lambda e: e.memset(epsc[:], EPS), w=[epsc])

    for l in range(nlayers):
        last = (l == DEPTH - 1)
        upd_ctx = not last
        ntl = NT if upd_ctx else 32

        sc.barrier(new_epoch=(l > 0))
        with contextlib.ExitStack() as ps_:
            wm = ring("wm", 2, [128, 8, 1536], BF16, ps_)
            dgf = ring("dgf", 2, [128, 128], F32, ps_)
            for t_, src in ((bmt, bmodT[l]), (n1t, n1g[l]), (n2t, n2g[l]), (qgt, qgc[l]), (kgt, kgc[l]),
                            (cwt, cwT[l]), (cbt, cbT[l]), (cgt, cgT[l])):
                dma("sp", t_[:], src, w=[t_])
            psM = PB[0]
            for cb in range(4):
                w_ = wm.next()
                dma("pool", w_[:], w_mod[l][:, cb * 1536:(cb + 1) * 1536].rearrange("(k p) n -> p k n", p=128), w=[w_])
                for j in range(12):
                    ch = cb * 12 + j
                    for k in range(8):
                        op("pe", lambda e: e.matmul(psM[:, ch * 2:ch * 2 + 2], lhsT=w_[:, k, j * 128:(j + 1) * 128],
                                                    rhs=scv[:, k, :], start=(k == 0), stop=(k == 7)),
                           r=[w_, scv], w=[psM])
            for i in range(2):
                op("dve", lambda e: e.tensor_tensor(out=mod[:, :, i], in0=psM[:, i:96:2], in1=bmt[:], op=ALU.add),
                   r=[psM, bmt], w=[mod])
            for (sct, c0, nt_) in ((sc1, 8, n1t), (sc2, 32, n2t)):
                for i in range(2):
                    op("dve", lambda e: e.scalar_tensor_tensor(out=sct[:, :, i], in0=mod[:, c0:c0 + 8, i], scalar=1.0,
                                                               in1=nt_[:], op0=ALU.add, op1=ALU.mult),
                       r=[mod, nt_], w=[sct])
            for a, c0 in ((0, 16), (1, 40)):
                for i in range(2):
                    for hf in range(2):
                        pg = PB[1 + hf]
                        for kk in range(4):
                            k = hf * 4 + kk
                            dg = dgf.next()
                            op("dve", lambda e: e.tensor_scalar(out=dg[:], in0=identf[:], scalar1=mod[:, c0 + k, i:i + 1],
                                                                scalar2=None, op0=ALU.mult), r=[identf, mod], w=[dg])
                            op("pe", lambda e: e.matmul(pg[:, kk * 128:(kk + 1) * 128], lhsT=onesf[:], rhs=dg[:],
                                                        start=True, stop=True), r=[onesf, dg], w=[pg])
                        op("act", lambda e: e.activation(out=gbc[a][i][:, hf * 512:(hf + 1) * 512], in_=pg[:], func=AF.Copy),
                           r=[pg], w=[gbc[a][i]])
        if l == 0:
            dbg("mod", mod, [128, 48, 2])
            dbg("sc1", sc1, [128, 8, 2])
            dbg("g1lat", gbc[0][0], [128, D])
            dbg("g2ctx", gbc[1][1], [128, D])
        if done(l, "M"):
            break

        sc.barrier()
        with contextlib.ExitStack() as ps_:
            PT = alloc_pt(ps_)
            hT = sb("hT", [128, 8, T], BF16, ps_)
            hTs = [Tile(hT.ap, "hT%d" % b_) for b_ in range(9)]
            rope = sb("rope", [128, 2, S], BF16, ps_)
            dma("pool", rope[:], k_rope, w=[rope])
            xr = ring("xr", 2, [128, D], F32, ps_)
            xnr = ring("xnr", 3, [128, D], BF16, ps_)
            ssr = ring("ssr", 3, [128, 4], F32, ps_)
            for t in range(NT):
                xt = xr.next()
                src, srct = xsrc(l, t)
                dma("sp", xt[:], src, r=[srct], w=[xt])
                norm_tile(PT, xt, hT, t * 128, sc1, 0, 0 if t < 32 else 1, xnr, ssr, trk=hTs[t // 4])

            wbuf = ring("wbuf", 2, [128, 8, 512], BF16, ps_)
            ob = ring("ob", 3, [128, 512], BF16, ps_)
            obv = ring("obv", 2, [128, 8, 128], BF16, ps_)
            sqb = ring("sqb", 2, [128, 512], BF16, ps_)
            rsf = ring("rsf", 2, [128, 512], F32, ps_)
            qnb = ring("qnb", 2, [128, 512], BF16, ps_)
            t1r = ring("t1r", 2, [128, 512], F32, ps_)
            t2r = ring("t2r", 2, [128, 512], F32, ps_)
            sgr = ring("sgr", 2, [128, 512], F32, ps_)
            for t_ in obv.tiles:
                op("dve", lambda e: e.memset(t_[:], 1.0), w=[t_])
            pmain = Ring(PB[0:4])
            blocks = [(b * 512, 512) for b in range(8)] + [(S, C)]

            def load_w(c0, n):
                w_ = wbuf.next()
                dma("pool", w_[:, :, 0:n], w_in[l][:, c0:c0 + n].rearrange("(k p) n -> p k n", p=128), w=[w_])
                return w_

            def mm_fm(ps, w_, wc0, t0, nt):
                for k in range(8):
                    op("pe", lambda e: e.matmul(ps[:, 0:nt], lhsT=w_[:, k, wc0:wc0 + 128], rhs=hT[:, k, t0:t0 + nt],
                                                start=(k == 0), stop=(k == 7)), r=[w_, hTs[t0 // 512]], w=[ps])

            def qk_prep(ps, nt, gcol, t0, dst, row0):
                is_lat = t0 < S
                sq = sqb.next()
                op("act", lambda e: e.activation(out=sq[:, 0:nt], in_=ps[:, 0:nt], func=AF.Square), r=[ps], w=[sq])
                pa = PB[4]
                op("pe", lambda e: e.matmul(pa[:, 0:nt], lhsT=bdones, rhs=sq[:, 0:nt], start=True, stop=True),
                   r=[c128, sq], w=[pa])
                rs = rsf.next()
                op("act", lambda e: e.activation(out=rs[:, 0:nt], in_=pa[:, 0:nt], func=AF.Sqrt, scale=1.0 / 64,
                                                 bias=epsc[:, 0:1]), r=[pa, epsc], w=[rs])
                op("dve", lambda e: e.reciprocal(out=rs[:, 0:nt], in_=rs[:, 0:nt]), r=[rs], w=[rs])
                qn = qnb.next()
                op("dve", lambda e: e.scalar_tensor_tensor(out=qn[:, 0:nt], in0=ps[:, 0:nt], scalar=gcol[:, 0:1],
                                                           in1=rs[:, 0:nt], op0=ALU.mult, op1=ALU.mult),
                   r=[ps, gcol, rs], w=[qn])
                if is_lat:
                    pb_ = PB[5]
                    op("pe", lambda e: e.matmul(pb_[:, 0:nt], lhsT=Pm, rhs=qn[:, 0:nt], start=True, stop=True),
                       r=[c128, qn], w=[pb_])
                    t1 = t1r.next()
                    t2 = t2r.next()
                    op("dve", lambda e: e.tensor_tensor(out=t1[:, 0:nt], in0=qn[:, 0:nt], in1=rope[:, 0, t0:t0 + nt],
                                                        op=ALU.mult), r=[qn, rope], w=[t1])
                    op("dve", lambda e: e.tensor_tensor(out=t2[:, 0:nt], in0=pb_[:, 0:nt], in1=rope[:, 1, t0:t0 + nt],
                                                        op=ALU.mult), r=[pb_, rope], w=[t2])
                    o = ob.next()
                    op("dve", lambda e: e.tensor_tensor(out=o[:, 0:nt], in0=t1[:, 0:nt], in1=t2[:, 0:nt], op=ALU.add),
                       r=[t1, t2], w=[o])
                    dma("sp", dst[row0:row0 + 128, t0:t0 + nt], o[:, 0:nt], r=[o], w=[dst])
                else:
                    dma("sp", dst[row0:row0 + 128, t0:t0 + nt], qn[:, 0:nt], r=[qn], w=[dst])

            w_ = load_w(0, 512)
            units = [(t0, nt, cch) for (t0, nt) in blocks for cch in range(4)]
            pend_ = None
            for (t0, nt, cch) in units:
                ps = pmain.next()
                mm_fm(ps, w_, cch * 128, t0, nt)
                if pend_ is not None:
                    qk_prep(*pend_)
                pend_ = (ps, nt, qgt, t0, QG, cch * 128)
            qk_prep(*pend_)
            w_ = wbuf.next()
            for h in range(2):
                for dup in range(2):
                    dma("pool", w_[:, :, h * 128 + dup * 64:h * 128 + dup * 64 + 64],
                        w_in[l][:, 512 + h * 64:512 + (h + 1) * 64].rearrange("(k p) n -> p k n", p=128), w=[w_])
            units = [(t0, nt, h) for (t0, nt) in blocks for h in range(2)]
            pend_ = None
            for (t0, nt, h) in units:
                ps = pmain.next()
                mm_fm(ps, w_, h * 128, t0, nt)
                if pend_ is not None:
                    qk_prep(*pend_)
                pend_ = (ps, nt, kgt, t0, KG, h * 128)
            qk_prep(*pend_)

            def mm_tm(ps, w_, n, t):
                for k in range(8):
                    op("pe", lambda e: e.matmul(ps[:, 0:n], lhsT=hT[:, k, t * 128:(t + 1) * 128], rhs=w_[:, k, 0:n],
                                                start=(k == 0), stop=(k == 7)), r=[w_, hTs[t // 4]], w=[ps])

            w_ = load_w(640, 128)
            for t in range(NT):
                ps = pmain.next()
                mm_tm(ps, w_, 128, t)
                o = obv.next()
                op("dve", lambda e: e.tensor_copy(out=o[:, 0:2, 0:64], in_=ps[:, 0:128].rearrange("p (h d) -> p h d", d=64)),
                   r=[ps], w=[o])
                dma("sp", VG[t * 128:(t + 1) * 128, :].rearrange("p (h d) -> p h d", d=128), o[:, 0:2, :], r=[o], w=[VG])
            for (c0, dst) in ((768, QN), (1280, KN)):
                w_ = load_w(c0, 512)
                for (t0, nt) in blocks:
                    for cch in range(4):
                        ps = pmain.next()
                        mm_fm(ps, w_, cch * 128, t0, nt)
                        o = ob.next()
                        op("act", lambda e: e.activation(out=o[:, 0:nt], in_=ps[:, 0:nt], func=AF.Copy), r=[ps], w=[o])
                        dma("sp", dst[cch * 128:(cch + 1) * 128, t0:t0 + nt], o[:, 0:nt], r=[o], w=[dst])
            w_ = load_w(1792, 512)
            for t in range(NT):
                ps = pmain.next()
                mm_tm(ps, w_, 512, t)
                o = obv.next()
                op("dve", lambda e: e.tensor_copy(out=o[:, :, 0:64], in_=ps[:, :].rearrange("p (h d) -> p h d", d=64)),
                   r=[ps], w=[o])
                dma("sp", VN[t * 128:(t + 1) * 128, :].rearrange("p (h d) -> p h d", d=128), o[:], r=[o], w=[VN])
            w_ = load_w(2304, 512)
            for t in range(NT):
                ps = pmain.next()
                mm_tm(ps, w_, 512, t)
                o = ob.next()
                op("act", lambda e: e.activation(out=o[:], in_=ps[:], func=AF.Copy), r=[ps], w=[o])
                dma("sp", U[t * 128:(t + 1) * 128, :], o[:], r=[o], w=[U])
            wa = load_w(2816, 512)
            wb_ = load_w(3328, 512)
            for (t0, nt) in blocks:
                for cch in range(4):
                    pa_ = pmain.next()
                    pb_ = pmain.next()
                    mm_fm(pa_, wa, cch * 128, t0, nt)
                    mm_fm(pb_, wb_, cch * 128, t0, nt)
                    sg_ = sgr.next()
                    op("act", lambda e: e.activation(out=sg_[:, 0:nt], in_=pb_[:, 0:nt], func=AF.Sigmoid), r=[pb_], w=[sg_])
                    o = ob.next()
                    op("dve", lambda e: e.tensor_tensor(out=o[:, 0:nt], in0=pa_[:, 0:nt], in1=sg_[:, 0:nt], op=ALU.mult),
                       r=[pa_, sg_], w=[o])
                    dma("sp", ZC[cch * 128:(cch + 1) * 128, t0:t0 + nt], o[:, 0:nt], r=[o], w=[ZC])
            nblk = len(blocks) if upd_ctx else 8
            for gq_ in range(8):
                w_ = load_w(3840 + gq_ * 512, 512)
                for (t0, nt) in blocks[:nblk]:
                    for cch in range(4):
                        ps = pmain.next()
                        mm_fm(ps, w_, cch * 128, t0, nt)
                        o = ob.next()
                        op("act", lambda e: e.activation(out=o[:, 0:nt], in_=ps[:, 0:nt], func=AF.Sigmoid), r=[ps], w=[o])
                        row0 = (gq_ * 4 + cch) * 128
                        dma("sp", SG[row0:row0 + 128, t0:t0 + nt], o[:, 0:nt], r=[o], w=[SG])
        if done(l, "P"):
            break

        def dense_heads(heads, nt, chunks, ptr, rtr, rdeps):
            n = len(chunks)

            def s_step(hd, jj):
                ps = hd["pS"][jj % len(hd["pS"])]
                op("pe", lambda e: e.matmul(ps[:, 0:nt], lhsT=hd["kfn"](chunks[jj]), rhs=hd["qap"], start=True, stop=True),
                   r=rdeps, w=[ps])
                return ps
            LK = min(len(heads[0]["pS"]) - 1, n)
            pend = [[s_step(hd, jj) for hd in heads] for jj in range(LK)]
            for jj in range(n):
                if jj + LK < n:
                    pend.append([s_step(hd, jj + LK) for hd in heads])
                cur = pend.pop(0)
                for hi, hd in enumerate(heads):
                    pt = ptr.next()
                    c_ = cur[hi]
                    op("act", lambda e: e.activation(out=pt[:, 0:nt], in_=c_[:, 0:nt], func=AF.Exp, scale=0.125),
                       r=[c_], w=[pt])
                    op("pe", lambda e: e.matmul(hd["acc"][:, 0:nt], lhsT=hd["vfn"](chunks[jj]), rhs=pt[:, 0:nt],
                                                start=(jj == 0), stop=(jj == n - 1)), r=rdeps + [pt], w=[hd["acc"]])
            for hd in heads:
                rt = rtr.next()
                acc = hd["acc"]
                op("dve", lambda e: e.reciprocal(out=rt[0:64, 0:nt], in_=acc[64:128, 0:nt]), r=[acc], w=[rt])
                op("dve", lambda e: e.tensor_tensor(out=hd["yap"], in0=acc[0:64, 0:nt], in1=rt[0:64, 0:nt],
                                                    op=ALU.mult), r=[acc, rt], w=[hd["ytile"]])

        sc.barrier()
        with contextlib.ExitStack() as ps_:
            PB7 = alloc_pb7(ps_)
            kgT = sb("kgT", [128, 2, T], BF16, ps_)
            vga = sb("vga", [128, NT, 2, 128], BF16, ps_)
            dma("sp", kgT[:], KG[:, :].rearrange("(h p) t -> p h t", p=128), r=[KG], w=[kgT])
            dma("sp", vga[:], VG[:, :].rearrange("(j p) (h d) -> p j h d", p=128, d=128), r=[VG], w=[vga])
            qtr = ring("qtr", 2, [128, 512], BF16, ps_)
            ptr = ring("ptr", 6, [128, 512], BF16, ps_)
            rtr = ring("rtr", 2, [64, 512], F32, ps_)
            ytr = ring("ytr", 4, [64, 512], BF16, ps_)
            accr = Ring([PB[6], PB7])
            blocks = [(b * 512, 512, list(range(NT))) for b in range(4 if last else 8)]
            if upd_ctx:
                blocks.append((S, C, [32, 33]))
            qt2r = ring("qt2r", 2, [128, 512], BF16, ps_)
            for (t0, nt, chunks) in blocks:
                for cch in range(4):
                    qt = qtr.next()
                    dma("sp", qt[:, 0:nt], QG[cch * 128:(cch + 1) * 128, t0:t0 + nt], r=[QG], w=[qt])
                    if last:
                        q2 = qt2r.next()
                        dma("sp", q2[:, 0:nt], QG[cch * 128:(cch + 1) * 128, t0 + S // 2:t0 + S // 2 + nt], r=[QG], w=[q2])
                        blend(qt, qt[:, 0:nt], q2, q2[:, 0:nt])
                    heads = []
                    for hp in range(2):
                        hq = cch * 2 + hp
                        kvh = hq // 4
                        P0 = hp * 64
                        yt = ytr.next()
                        heads.append(dict(qap=qt[P0:P0 + 64, 0:nt],
                                          kfn=(lambda j, P0=P0, kvh=kvh: kgT[P0:P0 + 64, kvh, j * 128:(j + 1) * 128]),
                                          vfn=(lambda j, kvh=kvh: vga[:, j, kvh, :]),
                                          pS=PB[3 * hp:3 * hp + 3], acc=accr.next(), ytile=yt, yap=yt[0:64, 0:nt], hq=hq))
                    dense_heads(heads, nt, chunks, ptr, rtr, [kgT, vga, qt])
                    for hd in heads:
                        hq = hd["hq"]
                        dma("sp", Y[hq * 64:(hq + 1) * 64, t0:t0 + nt], hd["ytile"][0:64, 0:nt], r=[hd["ytile"]], w=[Y])
                        if last:
                            dma("sp", Y[hq * 64:(hq + 1) * 64, t0 + S // 2:t0 + S // 2 + nt], hd["ytile"][0:64, 0:nt],
                                r=[hd["ytile"]], w=[Y])
        if done(l, "G"):
            break

        sc.barrier()
        with contextlib.ExitStack() as ps_:
            knT = sb("knT", [128, 4, T], BF16, ps_)
            vna = sb("vna", [128, NT, 8, 128], BF16, ps_)
            tst = sb("tst", [128, 8, 16, 64], BF16, ps_)
            dma("sp", knT[:], KN[:, :].rearrange("(c p) t -> p c t", p=128), r=[KN], w=[knT])
            dma("sp", vna[:], VN[:, :].rearrange("(j p) (h d) -> p j h d", p=128, d=128), r=[VN], w=[vna])
            namask = sb("namask", [128, 16, 64], BF16, ps_)
            dma("pool", namask[:], k_namask.rearrange("p (a b) -> p a b", b=64), w=[namask])
            rawt = [Tile(None, "raw%d" % p) for p in range(128)]
            for p in range(128):
                half, kc = divmod(p, 64)
                dma("pool", tst[p:p + 1, :, :, :], rpbP[l][:, half:half + 16, 63 - kc:63 - kc + 64], w=[rawt[p]])
            op("act", lambda e: e.activation(out=tst[:], in_=tst[:], func=AF.Exp), r=rawt, w=[tst])
            for h in range(8):
                op("dve", lambda e: e.tensor_tensor(out=tst[:, h], in0=tst[:, h], in1=namask[:], op=ALU.mult),
                   r=[tst, namask], w=[tst])
            qnr = ring("qnr", 2, [128, 4, 512], BF16, ps_)
            ptr = ring("ptr", 6, [128, 512], BF16, ps_)
            rtr = ring("rtr", 2, [64, 512], F32, ps_)
            ynr = ring("ynr", 2, [64, 8, 512], BF16, ps_)
            accr = Ring(PB[2:4])
            pS3 = [PB[0], PB[1], PB[4], PB[5]]
            LOOK = 3
            ucnt = [0]
            for b in range(8):
                qn = qnr.next()
                dma("sp", qn[:], QN[:, b * 512:(b + 1) * 512].rearrange("(c p) t -> p c t", p=128), r=[QN], w=[qn])
                yn = ynr.next()
                accs = [accr.next() for rl_ in range(8)]

                def na_S(rl, h, qn=qn, b=b):
                    r_ = b * 8 + rl
                    r0 = min(max(r_ - 4, 0), 56)
                    i0 = r0 // 2
                    nck = 4 if r0 % 2 == 0 else 5
                    chunks = [i0 + j for j in range(nck)] + [32, 33]
                    dri = 2 * i0 - r_ + 8
                    cch, P0 = h // 2, (h % 2) * 64
                    ps = pS3[ucnt[0] % len(pS3)]
                    ucnt[0] += 1
                    for jj, j in enumerate(chunks):
                        op("pe", lambda e: e.matmul(ps[:, jj * 64:(jj + 1) * 64], lhsT=knT[P0:P0 + 64, cch, j * 128:(j + 1) * 128],
                                                    rhs=qn[P0:P0 + 64, cch, rl * 64:(rl + 1) * 64], start=True, stop=True),
                           r=[knT, qn], w=[ps])
                    return (ps, rl, h, r0, nck, chunks, dri)

                def na_rest(u_, yn=yn, accs=accs):
                    ps, rl, h, r0, nck, chunks, dri = u_
                    ncol = (nck + 2) * 64
                    acc = accs[rl]
                    pt = ptr.next()
                    op("act", lambda e: e.activation(out=pt[:, 0:ncol], in_=ps[:, 0:ncol], func=AF.Exp, scale=0.125),
                       r=[ps], w=[pt])
                    op("pool", lambda e: e.tensor_tensor(out=pt[:, 0:nck * 64].rearrange("p (a b) -> p a b", b=64),
                                                         in0=pt[:, 0:nck * 64].rearrange("p (a b) -> p a b", b=64),
                                                         in1=tst[:, h, dri:dri + 2 * nck:2, :], op=ALU.mult),
                       r=[pt, tst], w=[pt])
                    if r0 % 2 == 1:
                        op("pool", lambda e: e.memset(pt[0:64, 0:64], 0.0), w=[pt])
                        op("pool", lambda e: e.memset(pt[64:128, (nck - 1) * 64:nck * 64], 0.0), w=[pt])
                    for jj, j in enumerate(chunks):
                        op("pe", lambda e: e.matmul(acc[:, h * 64:(h + 1) * 64], lhsT=vna[:, j, h, :],
                                                    rhs=pt[:, jj * 64:(jj + 1) * 64], start=(jj == 0),
                                                    stop=(jj == len(chunks) - 1)), r=[vna, pt], w=[acc])
                    if h == 7:
                        rt = rtr.next()
                        op("dve", lambda e: e.reciprocal(out=rt[0:64, :], in_=acc[64:128, :]), r=[acc], w=[rt])
                        op("dve", lambda e: e.tensor_tensor(out=yn[0:64, :, rl * 64:(rl + 1) * 64],
                                                            in0=acc[0:64, :].rearrange("p (h q) -> p h q", q=64),
                                                            in1=rt[0:64, :].rearrange("p (h q) -> p h q", q=64), op=ALU.mult),
                           r=[acc, rt], w=[yn])

                units = [(rl, h) for rl in range(8) for h in range(8)]
                pend = []
                for idx in range(len(units) + LOOK):
                    if idx < len(units):
                        pend.append(na_S(*units[idx]))
                    if idx >= LOOK:
                        na_rest(pend.pop(0))
                dma("sp", Y[512:1024, b * 512:(b + 1) * 512].rearrange("(h p) t -> p h t", p=64), yn[0:64, :, :], r=[yn], w=[Y])
            if upd_ctx:
                qn = qnr.next()
                dma("sp", qn[:, :, 0:C], QN[:, S:T].rearrange("(c p) t -> p c t", p=128), r=[QN], w=[qn])
                yn = ynr.next()
                acc3 = Ring([PB[6], PB[2], PB[3]])
                for cch in range(4):
                    heads = []
                    for hp in range(2):
                        h = cch * 2 + hp
                        P0 = hp * 64
                        heads.append(dict(qap=qn[P0:P0 + 64, cch, 0:C],
                                          kfn=(lambda j, P0=P0, cch=cch: knT[P0:P0 + 64, cch, j * 128:(j + 1) * 128]),
                                          vfn=(lambda j, h=h: vna[:, j, h, :]),
                                          pS=[PB[0], PB[1]] if hp == 0 else [PB[4], PB[5]], acc=acc3.next(), ytile=yn,
                                          yap=yn[0:64, h, 0:C]))
                    dense_heads(heads, C, [32, 33], ptr, rtr, [knT, vna, qn])
                dma("sp", Y[512:1024, S:T].rearrange("(h p) t -> p h t", p=64), yn[0:64, :, 0:C], r=[yn], w=[Y])
        if done(l, "A"):
            break

        sc.barrier()
        with contextlib.ExitStack() as ps_:
            dft256 = sb("dft256", [128, 2, 2, 256], BF16, ps_)
            f1 = sb("f1", [64, 128], BF16, ps_)
            m12 = sb("m12", [64, 2, 64, 128], BF16, ps_)
            dma("pool", dft256[:], k_dft256, w=[dft256])
            dma("pool", f1[:], k_f1, w=[f1])
            dma("pool", m12[:], k_m12.rearrange("p a (b c) -> p a b c", c=128), w=[m12])
            u1r = ring("u1r", 2, [64, 64, 128], BF16, ps_)
            a1 = sb("a1", [64, 128, 128], BF16, ps_)
            xt_ = sb("xtf", [128, 2, S], BF16, ps_)
            yo = ring("yo", 2, [128, 512], BF16, ps_)
            pr = Ring(PB[0:4])
            sclat = 1.0 / float(np.sqrt(S * 128.0))
            for g in range(4):
                u1 = u1r.next()
                dma("sp", u1[:], U[0:S, g * 128:(g + 1) * 128].rearrange("(a b) c -> a b c", b=64), r=[U], w=[u1])
                for c4 in range(32):
                    ps = pr.next()
                    for cc in range(4):
                        c = c4 * 4 + cc
                        op("pe", lambda e: e.matmul(ps[0:64, cc * 128:(cc + 1) * 128], lhsT=u1[:, :, c], rhs=f1[:],
                                                    start=True, stop=True), r=[u1, f1], w=[ps])
                    eng = "act" if c4 % 2 == 0 else "dve"
                    if eng == "act":
                        op("act", lambda e: e.activation(out=a1[:, c4 * 4:c4 * 4 + 4, :],
                                                         in_=ps[0:64, :].rearrange("p (a b) -> p a b", b=128), func=AF.Copy),
                           r=[ps], w=[a1])
                    else:
                        op("dve", lambda e: e.tensor_copy(out=a1[:, c4 * 4:c4 * 4 + 4, :],
                                                          in_=ps[0:64, :].rearrange("p (a b) -> p a b", b=128)),
                           r=[ps], w=[a1])
                for k4 in range(16):
                    ps = pr.next()
                    for kk in range(4):
                        k1 = k4 * 4 + kk
                        op("pe", lambda e: e.matmul(ps[:, kk * 128:(kk + 1) * 128], lhsT=a1[:, :, k1], rhs=m12[:, 0, k1, :],
                                                    start=True, stop=False), r=[a1, m12], w=[ps])
                        op("pe", lambda e: e.matmul(ps[:, kk * 128:(kk + 1) * 128], lhsT=a1[:, :, 64 + k1], rhs=m12[:, 1, k1, :],
                                                    start=False, stop=True), r=[a1, m12], w=[ps])
                    for ri in range(2):
                        src = ps[:, :].rearrange("p (a r k) -> p a r k", a=4, r=2)[:, :, ri, :]
                        dst = xt_[:, ri, :].rearrange("p (k a) -> p a k", a=64)[:, k4 * 4:k4 * 4 + 4, :]
                        if ri == 0:
                            op("act", lambda e: e.activation(out=dst, in_=src, func=AF.Copy), r=[ps], w=[xt_])
                        else:
                            op("dve", lambda e: e.tensor_copy(out=dst, in_=src), r=[ps], w=[xt_])
                for b in range(8):
                    ps = pr.next()
                    op("pe", lambda e: e.matmul(ps[:], lhsT=CCm, rhs=xt_[:, 0, b * 512:(b + 1) * 512], start=True, stop=False),
                       r=[c128, xt_], w=[ps])
                    op("pe", lambda e: e.matmul(ps[:], lhsT=SCm, rhs=xt_[:, 1, b * 512:(b + 1) * 512], start=False, stop=True),
                       r=[c128, xt_], w=[ps])
                    o = yo.next()
                    op("act", lambda e: e.activation(out=o[:], in_=ps[:], func=AF.Copy, scale=sclat), r=[ps], w=[o])
                    dma("sp", Y[1024 + g * 128:1024 + (g + 1) * 128, b * 512:(b + 1) * 512], o[:], r=[o], w=[Y])
            if upd_ctx:
                uc = sb("uc", [128, 2, 512], BF16, ps_)
                xc_ = sb("xcf", [128, 2, C], BF16, ps_)
                dma("sp", uc[:], U[S:T, :].rearrange("(n p) c -> p n c", p=128), r=[U], w=[uc])
                scc = 1.0 / float(np.sqrt(C * 128.0))
                for g in range(4):
                    ps = pr.next()
                    for ri in range(2):
                        for n in range(2):
                            op("pe", lambda e: e.matmul(ps[:, ri * C:(ri + 1) * C], lhsT=uc[:, n, g * 128:(g + 1) * 128],
                                                        rhs=dft256[:, ri, n, :], start=(n == 0), stop=(n == 1)),
                               r=[uc, dft256], w=[ps])
                    op("act", lambda e: e.activation(out=xc_[:], in_=ps[:, :].rearrange("p (r k) -> p r k", r=2), func=AF.Copy),
                       r=[ps], w=[xc_])
                    ps2 = pr.next()
                    op("pe", lambda e: e.matmul(ps2[:, 0:C], lhsT=CCm, rhs=xc_[:, 0, :], start=True, stop=False),
                       r=[c128, xc_], w=[ps2])
                    op("pe", lambda e: e.matmul(ps2[:, 0:C], lhsT=SCm, rhs=xc_[:, 1, :], start=False, stop=True),
                       r=[c128, xc_], w=[ps2])
                    o = yo.next()
                    op("act", lambda e: e.activation(out=o[:, 0:C], in_=ps2[:, 0:C], func=AF.Copy, scale=scc), r=[ps2], w=[o])
                    dma("sp", Y[1024 + g * 128:1024 + (g + 1) * 128, S:T], o[:, 0:C], r=[o], w=[Y])
        if done(l, "F"):
            break

        sc.barrier()
        with contextlib.ExitStack() as ps_:
            dg = sb("dg", [128, 4, 31, 128], BF16, ps_)
            for cch in range(4):
                for k in range(31):
                    op("dve", lambda e: e.tensor_scalar(out=dg[:, cch, k, :], in0=ident, scalar1=cwt[:, cch, k:k + 1],
                                                        scalar2=None, op0=ALU.mult), r=[c128, cwt], w=[dg])
            zc = sb("zc", [128, 4, S + 30], BF16, ps_)
            zcc = sb("zcc", [128, 4, C + 30], BF16, ps_)
            ycr = ring("ycr", 2, [128, 4, 512], F32, ps_)
            sqr = ring("sqr", 2, [128, 4, 512], BF16, ps_)
            rsr = ring("rsr", 2, [128, 512], F32, ps_)
            tmr = ring("tmr", 2, [128, 512], F32, ps_)
            yo = ring("yoc", 3, [128, 512], BF16, ps_)
            seqs = [(zc, 0, S)] + ([(zcc, S, C)] if upd_ctx else [])
            for (zt, s0, L) in seqs:
                op("dve", lambda e: e.memset(zt[:, :, 0:15], 0.0), w=[zt])
                op("dve", lambda e: e.memset(zt[:, :, 15 + L:30 + L], 0.0), w=[zt])
                dma("sp", zt[:, :, 15:15 + L], ZC[:, s0:s0 + L].rearrange("(c p) t -> p c t", p=128), r=[ZC], w=[zt])
                nt = min(512, L)
                for b in range(L // nt):
                    t0 = b * nt
                    yc = ycr.next()
                    sq = sqr.next()
                    for cch in range(4):
                        ps = PB[cch]
                        for k in range(31):
                            op("pe", lambda e: e.matmul(ps[:, 0:nt], lhsT=dg[:, cch, k, :], rhs=zt[:, cch, t0 + k:t0 + k + nt],
                                                        start=(k == 0), stop=(k == 30)), r=[dg, zt], w=[ps])
                        op("act", lambda e: e.activation(out=yc[:, cch, 0:nt], in_=ps[:, 0:nt], func=AF.Identity,
                                                         bias=cbt[:, cch:cch + 1]), r=[ps, cbt], w=[yc])
                        op("act", lambda e: e.activation(out=sq[:, cch, 0:nt], in_=ps[:, 0:nt], func=AF.Square,
                                                         bias=cbt[:, cch:cch + 1]), r=[ps, cbt], w=[sq])
                    pss = PB[4]
                    for cch in range(4):
                        op("pe", lambda e: e.matmul(pss[:, 0:nt], lhsT=ones, rhs=sq[:, cch, 0:nt], start=(cch == 0),
                                                    stop=(cch == 3)), r=[c128, sq], w=[pss])
                    rs = rsr.next()
                    op("act", lambda e: e.activation(out=rs[:, 0:nt], in_=pss[:, 0:nt], func=AF.Sqrt, scale=1.0 / 512,
                                                     bias=epsc[:, 0:1]), r=[pss, epsc], w=[rs])
                    op("dve", lambda e: e.reciprocal(out=rs[:, 0:nt], in_=rs[:, 0:nt]), r=[rs], w=[rs])
                    for cch in range(4):
                        tm = tmr.next()
                        op("dve", lambda e: e.tensor_tensor(out=tm[:, 0:nt], in0=yc[:, cch, 0:nt], in1=rs[:, 0:nt], op=ALU.mult),
                           r=[yc, rs], w=[tm])
                        o = yo.next()
                        op("act", lambda e: e.activation(out=o[:, 0:nt], in_=tm[:, 0:nt], func=AF.Silu,
                                                         scale=cgt[:, cch:cch + 1]), r=[tm, cgt], w=[o])
                        dma("sp", Y[1536 + cch * 128:1536 + (cch + 1) * 128, s0 + t0:s0 + t0 + nt], o[:, 0:nt], r=[o], w=[Y])
        if done(l, "C"):
            break

        sc.barrier()
        with contextlib.ExitStack() as ps_:
            wbr = sb("wbr", [128, 16, D], BF16, ps_)
            wo = sb("wo", [128, 8, D], BF16, ps_)
            for n in range(4):
                dma("pool", wbr[:, n * 4:(n + 1) * 4, :], w_branch[l, n].rearrange("(k p) d -> p k d", p=128), w=[wbr])
            dma("pool", wo[:], w_out[l].rearrange("(k p) d -> p k d", p=128), w=[wo])
            ybr = ring("ybr", 2, [128, 16, 512], BF16, ps_)
            sgr = ring("sgb", 3, [128, 4, 512], BF16, ps_)
            accf = ring("accf", 2, [128, 512], F32, ps_)
            tmpf = ring("tmpf", 3, [128, 512], F32, ps_)
            mT = ring("mT", 2, [128, 8, 512], BF16, ps_)
            xr = ring("xr2", 3, [128, D], F32, ps_)
            xo = ring("xo2", 3, [128, D], F32, ps_)
            tmx = ring("tmx", 2, [128, 512], F32, ps_)
            pz = Ring(PB[0:3])
            po = Ring(PB[3:7])
            blocks = [(b * 512, 512) for b in range(4 if last else 8)] + ([(S, C)] if upd_ctx else [])
            if last:
                yb2r = ring("yb2r", 1, [128, 16, 512], BF16, ps_)
                sg2r = ring("sg2r", 2, [128, 4, 512], BF16, ps_)
                x2r = ring("x2r", 2, [128, D], F32, ps_)
            H = S // 2
            def merge_part(t0, nt):
                i = 0 if t0 < S else 1
                yb = ybr.next()
                dma("sp", yb[:, :, 0:nt], Y[:, t0:t0 + nt].rearrange("(j p) t -> p j t", p=128), r=[Y], w=[yb])
                if last:
                    yb2 = yb2r.next()
                    dma("sp", yb2[:, :, 0:nt], Y[:, t0 + H:t0 + H + nt].rearrange("(j p) t -> p j t", p=128), r=[Y], w=[yb2])
                    blend(yb, yb[:, :, 0:nt], yb2, yb2[:, :, 0:nt])
                m_ = mT.next()
                for dc in range(8):
                    sg_ = sgr.next()
                    dma("sp", sg_[:, :, 0:nt],
                        SG[:, t0:t0 + nt].rearrange("(n j p) t -> p n j t", p=128, j=8)[:, :, dc, :], r=[SG], w=[sg_])
                    if last:
                        sg2 = sg2r.next()
                        dma("sp", sg2[:, :, 0:nt],
                            SG[:, t0 + H:t0 + H + nt].rearrange("(n j p) t -> p n j t", p=128, j=8)[:, :, dc, :], r=[SG], w=[sg2])
                        blend(sg_, sg_[:, :, 0:nt], sg2, sg2[:, :, 0:nt])
                    af = accf.next()
                    for n in range(4):
                        ps = pz.next()
                        for kc in range(4):
                            op("pe", lambda e: e.matmul(ps[:, 0:nt], lhsT=wbr[:, n * 4 + kc, dc * 128:(dc + 1) * 128],
                                                        rhs=yb[:, n * 4 + kc, 0:nt], start=(kc == 0), stop=(kc == 3)),
                               r=[wbr, yb], w=[ps])
                        if n == 0:
                            op("dve", lambda e: e.tensor_tensor(out=af[:, 0:nt], in0=ps[:, 0:nt], in1=sg_[:, n, 0:nt],
                                                                op=ALU.mult), r=[ps, sg_], w=[af])
                        else:
                            tf = tmpf.next()
                            op("dve", lambda e: e.tensor_tensor(out=tf[:, 0:nt], in0=ps[:, 0:nt], in1=sg_[:, n, 0:nt],
                                                                op=ALU.mult), r=[ps, sg_], w=[tf])
                            if n < 3:
                                op("pool", lambda e: e.tensor_tensor(out=af[:, 0:nt], in0=af[:, 0:nt], in1=tf[:, 0:nt], op=ALU.add),
                                   r=[af, tf], w=[af])
                            else:
                                op("pool", lambda e: e.tensor_tensor(out=m_[:, dc, 0:nt], in0=af[:, 0:nt], in1=tf[:, 0:nt],
                                                                     op=ALU.add), r=[af, tf], w=[m_])
                return (t0, nt, i, m_)

            def out_part(ctx_):
                t0, nt, i, m_ = ctx_
                for tt in range(nt // 128):
                    t = t0 // 128 + tt
                    xt = xr.next()
                    src, srct = xsrc(l, t)
                    dma("sp", xt[:], src, r=[srct], w=[xt])
                    if last:
                        xt2 = x2r.next()
                        dma("sp", xt2[:], X[(t + 16) * 128:(t + 17) * 128, :], r=[X], w=[xt2])
                        blend(xt, xt[:], xt2, xt2[:])
                    xn_ = xo.next()
                    for hf in range(2):
                        ps = po.next()
                        for dc in range(8):
                            op("pe", lambda e: e.matmul(ps[:], lhsT=m_[:, dc, tt * 128:(tt + 1) * 128],
                                                        rhs=wo[:, dc, hf * 512:(hf + 1) * 512], start=(dc == 0), stop=(dc == 7)),
                               r=[m_, wo], w=[ps])
                        tx = tmx.next()
                        op("dve", lambda e: e.tensor_tensor(out=tx[:], in0=ps[:], in1=gbc[0][i][:, hf * 512:(hf + 1) * 512],
                                                            op=ALU.mult), r=[ps, gbc[0][i]], w=[tx])
                        op("pool", lambda e: e.tensor_tensor(out=xn_[:, hf * 512:(hf + 1) * 512], in0=tx[:],
                                                             in1=xt[:, hf * 512:(hf + 1) * 512], op=ALU.add), r=[tx, xt], w=[xn_])
                    if last:
                        dma("sp", X2[t * 128:(t + 1) * 128, :], xn_[:], r=[xn_], w=[X2])
                    else:
                        dma("sp", X[t * 128:(t + 1) * 128, :], xn_[:], r=[xn_], w=[X])
            ctxs = []
            for bi in range(len(blocks) + 1):
                if bi < len(blocks):
                    ctxs.append(merge_part(*blocks[bi]))
                if bi >= 1:
                    out_part(ctxs[bi - 1])
        if done(l, "R"):
            break

        sc.barrier()
        with contextlib.ExitStack() as ps_:
            groups = [list(range(0, 12)), list(range(12, 24)), list(range(24, ntl))] if not last else [list(range(0, 8)), list(range(8, 16))]
            XE = X2 if last else X
            maxg = max(len(g_) for g_ in groups)
            PT = alloc_pt(ps_)
            h2T = sb("h2T", [128, 8, maxg * 128], BF16, ps_)
            h2Ts = [Tile(h2T.ap, "h2T%d" % b_) for b_ in range(3)]
            gate = sb("gate", [128, maxg, NE], F32, ps_)
            acc = sb("accm", [128, maxg, D], F32, ps_)
            xr = ring("xr3", 2, [128, D], F32, ps_)
            xnr = ring("xnr3", 3, [128, D], BF16, ps_)
            ssr = ring("ssr3", 3, [128, 4], F32, ps_)
            wgu = ring("wgu", 2, [128, 8, D], BF16, ps_)
            wdn = ring("wdn", 2, [128, 4, D], BF16, ps_)
            silr = ring("silr", 2, [128, 512], F32, ps_)
            hmr = ring("hmr", 2, [128, 4, 512], BF16, ps_)
            xo = ring("xo3", 2, [128, D], F32, ps_)
            gS = sb("gS", [128, maxg, NE], F32, ps_)
            gSel = sb("gSel", [128, maxg, NE], F32, ps_)
            gA = sb("gA", [128, maxg, NE], F32, ps_)
            gB = sb("gB", [128, maxg, NE], F32, ps_)
            gM = [sb("gM%d" % i_, [128, maxg * 4], F32, ps_) for i_ in range(4)]
            gT = sb("gT", [128, 4, maxg], F32, ps_)
            pgu = Ring(PB[0:4])
            pdn = Ring(PB[4:7])
            for grp in groups:
                ng = len(grp)
                for ti, t in enumerate(grp):
                    i = 0 if t < 32 else 1
                    xt = xr.next()
                    dma("sp", xt[:], XE[t * 128:(t + 1) * 128, :], r=[XE], w=[xt])
                    norm_tile(PT, xt, h2T, ti * 128, sc2, 24, i, xnr, ssr, trk=h2Ts[ti // 4])
                    pr_ = PB[5 + (ti % 2)]
                    for k in range(8):
                        op("pe", lambda e: e.matmul(pr_[:, 0:NE], lhsT=h2T[:, k, ti * 128:(ti + 1) * 128], rhs=wr[:, k, :],
                                                    start=(k == 0), stop=(k == 7)), r=[h2Ts[ti // 4], wr], w=[pr_])
                    op("act", lambda e: e.activation(out=gS[:, ti, :], in_=pr_[:, 0:NE], func=AF.Sigmoid), r=[pr_], w=[gS])
                G4 = ng * 4
                v16 = lambda t_: t_[:, 0:ng, :]
                v44 = lambda t_: t_[:, 0:ng, :].rearrange("p g (a e) -> p (g a) e", e=4)
                flat = lambda t_: t_[:, 0:ng, :].rearrange("p g e -> p (g e)")
                m1, m2, gs, pen = gM
                op("dve", lambda e: e.tensor_tensor(out=v16(gSel), in0=v16(gS), in1=rbt[:, 0:ng, :], op=ALU.add), r=[gS, rbt], w=[gSel])
                op("dve", lambda e: e.tensor_reduce(out=m1[:, 0:G4], in_=v44(gSel), axis=AX.X, op=ALU.max), r=[gSel], w=[m1])
                op("dve", lambda e: e.tensor_tensor(out=v44(gA), in0=v44(gSel), in1=m1[:, 0:G4].unsqueeze(2).broadcast_to([128, G4, 4]),
                                                    op=ALU.is_equal), r=[gSel, m1], w=[gA])
                op("dve", lambda e: e.scalar_tensor_tensor(out=flat(gB), in0=flat(gA), scalar=-BIG, in1=flat(gSel), op0=ALU.mult,
                                                           op1=ALU.add), r=[gA, gSel], w=[gB])
                op("dve", lambda e: e.tensor_reduce(out=m2[:, 0:G4], in_=v44(gB), axis=AX.X, op=ALU.max), r=[gB], w=[m2])
                op("dve", lambda e: e.tensor_tensor(out=gs[:, 0:G4], in0=m1[:, 0:G4], in1=m2[:, 0:G4], op=ALU.add), r=[m1, m2], w=[gs])
                op("dve", lambda e: e.tensor_reduce(out=gT[:, 0, 0:ng], in_=gs[:, 0:G4].rearrange("p (g a) -> p g a", a=4), axis=AX.X,
                                                    op=ALU.max), r=[gs], w=[gT])
                op("dve", lambda e: e.tensor_tensor(out=pen[:, 0:G4].rearrange("p (g a) -> p g a", a=4),
                                                    in0=gs[:, 0:G4].rearrange("p (g a) -> p g a", a=4),
                                                    in1=gT[:, 0, 0:ng].unsqueeze(2).broadcast_to([128, ng, 4]), op=ALU.is_equal),
                   r=[gs, gT], w=[pen])
                op("dve", lambda e: e.tensor_scalar(out=pen[:, 0:G4], in0=pen[:, 0:G4], scalar1=-1.0, scalar2=BIG, op0=ALU.add,
                                                    op1=ALU.mult), r=[pen], w=[pen])
                op("dve", lambda e: e.tensor_tensor(out=v44(gA), in0=v44(gSel), in1=pen[:, 0:G4].unsqueeze(2).broadcast_to([128, G4, 4]),
                                                    op=ALU.add), r=[gSel, pen], w=[gA])
                op("dve", lambda e: e.tensor_reduce(out=gT[:, 1, 0:ng], in_=v16(gA), axis=AX.X, op=ALU.max), r=[gA], w=[gT])
                op("dve", lambda e: e.tensor_tensor(out=v16(gB), in0=v16(gA), in1=gT[:, 1, 0:ng].unsqueeze(2).broadcast_to([128, ng, NE]),
                                                    op=ALU.is_equal), r=[gA, gT], w=[gB])
                op("dve", lambda e: e.scalar_tensor_tensor(out=flat(gSel), in0=flat(gB), scalar=-BIG, in1=flat(gA), op0=ALU.mult,
                                                           op1=ALU.add), r=[gB, gA], w=[gSel])
                op("dve", lambda e: e.tensor_reduce(out=gT[:, 2, 0:ng], in_=v16(gSel), axis=AX.X, op=ALU.max), r=[gSel], w=[gT])
                op("dve", lambda e: e.tensor_tensor(out=v16(gA), in0=v16(gSel), in1=gT[:, 2, 0:ng].unsqueeze(2).broadcast_to([128, ng, NE]),
                                                    op=ALU.is_equal), r=[gSel, gT], w=[gA])
                op("dve", lambda e: e.tensor_tensor(out=flat(gA), in0=flat(gA), in1=flat(gB), op=ALU.add), r=[gA, gB], w=[gA])
                op("dve", lambda e: e.tensor_tensor(out=flat(gB), in0=flat(gA), in1=flat(gS), op=ALU.mult), r=[gA, gS], w=[gB])
                op("dve", lambda e: e.tensor_reduce(out=gT[:, 3, 0:ng], in_=v16(gB), axis=AX.X, op=ALU.add), r=[gB], w=[gT])
                op("dve", lambda e: e.reciprocal(out=gT[:, 3, 0:ng], in_=gT[:, 3, 0:ng]), r=[gT], w=[gT])
                op("dve", lambda e: e.tensor_tensor(out=gate[:, 0:ng, :], in0=v16(gB),
                                                    in1=gT[:, 3, 0:ng].unsqueeze(2).broadcast_to([128, ng, NE]), op=ALU.mult),
                   r=[gB, gT], w=[gate])
                nblk = (ng + 3) // 4
                def gu_part(ex, b, wts):
                    if b == 0:
                        wg_ = wgu.next()
                        wd_ = wdn.next()
                        dma("pool", wg_[:], w_gu[l, ex].rearrange("(k p) n -> p k n", p=128), w=[wg_])
                        dma("pool", wd_[:], w_dn[l, ex].rearrange("(k p) n -> p k n", p=128), w=[wd_])
                        wts[ex] = (wg_, wd_)
                    wg_, wd_ = wts[ex]
                    tl0 = b * 4
                    ntile = min(4, ng - tl0)
                    nt = ntile * 128
                    c0 = tl0 * 128
                    hm = hmr.next()
                    for cch in range(4):
                        pa_ = pgu.next()
                        pb_ = pgu.next()
                        for k in range(8):
                            op("pe", lambda e: e.matmul(pa_[:, 0:nt], lhsT=wg_[:, k, cch * 128:(cch + 1) * 128],
                                                        rhs=h2T[:, k, c0:c0 + nt], start=(k == 0), stop=(k == 7)),
                               r=[wg_, h2Ts[b]], w=[pa_])
                        for k in range(8):
                            op("pe", lambda e: e.matmul(pb_[:, 0:nt], lhsT=wg_[:, k, 512 + cch * 128:512 + (cch + 1) * 128],
                                                        rhs=h2T[:, k, c0:c0 + nt], start=(k == 0), stop=(k == 7)),
                               r=[wg_, h2Ts[b]], w=[pb_])
                        sl = silr.next()
                        op("act", lambda e: e.activation(out=sl[:, 0:nt], in_=pa_[:, 0:nt], func=AF.Silu), r=[pa_], w=[sl])
                        op("dve", lambda e: e.tensor_tensor(out=hm[:, cch, 0:nt], in0=pb_[:, 0:nt], in1=sl[:, 0:nt],
                                                            op=ALU.mult), r=[pb_, sl], w=[hm])
                    return (ex, tl0, ntile, hm, wd_)

                def dn_part(ctx_):
                    ex, tl0, ntile, hm, wd_ = ctx_
                    for tt in range(ntile):
                        ti = tl0 + tt
                        for hf in range(2):
                            pd = pdn.next()
                            for cch in range(4):
                                op("pe", lambda e: e.matmul(pd[:], lhsT=hm[:, cch, tt * 128:(tt + 1) * 128],
                                                            rhs=wd_[:, cch, hf * 512:(hf + 1) * 512], start=(cch == 0),
                                                            stop=(cch == 3)), r=[hm, wd_], w=[pd])
                            if ex == 0:
                                op("dve", lambda e: e.tensor_scalar(out=acc[:, ti, hf * 512:(hf + 1) * 512], in0=pd[:],
                                                                    scalar1=gate[:, ti, ex:ex + 1], scalar2=None,
                                                                    op0=ALU.mult), r=[pd, gate], w=[acc])
                            else:
                                op("dve", lambda e: e.scalar_tensor_tensor(out=acc[:, ti, hf * 512:(hf + 1) * 512], in0=pd[:],
                                                                           scalar=gate[:, ti, ex:ex + 1],
                                                                           in1=acc[:, ti, hf * 512:(hf + 1) * 512],
                                                                           op0=ALU.mult, op1=ALU.add),
                                   r=[pd, gate, acc], w=[acc])
                wts = {}
                pend = None
                for ex in range(NE):
                    for b in range(nblk):
                        cur_ = gu_part(ex, b, wts)
                        if pend is not None:
                            dn_part(pend)
                        pend = cur_
                dn_part(pend)
                for ti, t in enumerate(grp):
                    i = 0 if t < 32 else 1
                    xt = xr.next()
                    dma("sp", xt[:], XE[t * 128:(t + 1) * 128, :], r=[XE], w=[xt])
                    xn_ = xo.next()
                    op("dve", lambda e: e.tensor_tensor(out=acc[:, ti, :], in0=acc[:, ti, :], in1=gbc[1][i][:], op=ALU.mult),
                       r=[acc, gbc[1][i]], w=[acc])
                    op("dve", lambda e: e.tensor_tensor(out=xn_[:], in0=acc[:, ti, :], in1=xt[:], op=ALU.add), r=[acc, xt], w=[xn_])
                    if not last:
                        dma("sp", X[t * 128:(t + 1) * 128, :], xn_[:], r=[xn_], w=[X])
                    else:
                        junk = xnr.next()
                        ss = ssr.next()
                        op("act", lambda e: e.activation(out=junk[:], in_=xn_[:], func=AF.Square, accum_out=ss[:, 0:1]),
                           r=[xn_], w=[junk, ss])
                        op("act", lambda e: e.activation(out=ss[:, 1:2], in_=ss[:, 0:1], func=AF.Sqrt, scale=1.0 / D,
                                                         bias=epsc[:, 0:1]), r=[ss, epsc], w=[ss])
                        op("dve", lambda e: e.reciprocal(out=ss[:, 2:3], in_=ss[:, 1:2]), r=[ss], w=[ss])
                        xf = xr.next()
                        op("dve", lambda e: e.scalar_tensor_tensor(out=xf[:], in0=xn_[:], scalar=ss[:, 2:3], in1=fngt[:],
                                                                   op0=ALU.mult, op1=ALU.mult), r=[xn_, ss, fngt], w=[xf])
                        dma("sp", out_d[t * 128:(t + 1) * 128, :], xf[:], r=[xf], w=[X])
        if done(l, "E"):
            break

    sc.barrier()


def _const_tables():
    p = np.arange(128)
    ident = np.eye(128, dtype=np.float32)
    ones = np.ones((128, 128), np.float32)
    bd = np.kron(np.eye(2, dtype=np.float32), np.ones((64, 64), np.float32))
    Pm = np.zeros((128, 128), np.float32)
    for i in range(64):
        Pm[2 * i + 1, 2 * i] = -1.0
        Pm[2 * i, 2 * i + 1] = 1.0
    ang = 2.0 * np.pi * np.outer(p, p) / 128.0
    CC = np.cos(ang).astype(np.float32)
    SC = np.sin(ang).astype(np.float32)
    c128 = np.concatenate([ident, ones, bd, Pm, CC, SC], axis=1)
    t = np.arange(S)
    row = (t // 64).astype(np.float32)
    col = (t % 64).astype(np.float32)
    inv = (10000.0 ** (-np.arange(16, dtype=np.float32) / 16)).astype(np.float32)
    angt = np.concatenate([row[:, None] * inv, col[:, None] * inv], axis=-1).astype(np.float32)
    j = (p % 64) // 2
    rope = np.stack([np.cos(angt)[:, j].T, np.sin(angt)[:, j].T], axis=1).astype(np.float32)
    kc = p % 64
    half = p // 64
    qc = np.arange(64)
    c0 = np.clip(qc - 8, 0, 48)
    colm = ((kc[:, None] >= c0[None, :]) & (kc[:, None] < c0[None, :] + 16)).astype(np.float32)
    dr = np.arange(16) - 8
    drv = ((dr[None, :] + half[:, None] >= -7) & (dr[None, :] + half[:, None] <= 7)).astype(np.float32)
    namask = (drv[:, :, None] * colm[:, None, :]).reshape(128, 16 * 64).astype(np.float32)
    n = np.arange(256)
    a256 = 2.0 * np.pi * np.outer(n, n) / 256.0
    d256 = np.stack([np.cos(a256), -np.sin(a256)], 0).reshape(2, 2, 128, 256).transpose(2, 0, 1, 3).astype(np.float32)
    n1 = np.arange(64)
    a64 = 2.0 * np.pi * np.outer(n1, n1) / 64.0
    f1 = np.concatenate([np.cos(a64), -np.sin(a64)], axis=1).astype(np.float32)
    k1 = np.arange(64)
    k2 = np.arange(64)
    kk = k1[:, None] + 64 * k2[None, :]
    am = 2.0 * np.pi * n1[:, None, None] * kk[None, :, :] / 4096.0
    Mre = np.cos(am)
    Mim = -np.sin(am)
    M1 = np.concatenate([Mre, Mim], axis=2)
    M2 = np.concatenate([-Mim, Mre], axis=2)
    m12 = np.stack([M1, M2], axis=1).reshape(64, 2, 64 * 128).astype(np.float32)
    return dict(c128=c128, rope=rope, namask=namask, dft256=np.ascontiguousarray(d256), f1=f1, m12=m12)


def _col(v, k):
    return np.ascontiguousarray(v.reshape(k, 128).T)


def _prep_inputs(inp):
    f = lambda a: np.ascontiguousarray(np.asarray(a, dtype=np.float32))
    shared = dict(_const_tables())
    shared["w_mod"] = f(inp["w_mod"])
    shared["bmodT"] = np.stack([_col(f(inp["b_mod"])[l], 48) for l in range(DEPTH)])
    shared["n1g"] = np.stack([_col(f(inp["norm1_g"])[l], 8) for l in range(DEPTH)])
    shared["n2g"] = np.stack([_col(f(inp["norm2_g"])[l], 8) for l in range(DEPTH)])
    shared["w_in"] = f(inp["w_in"])
    shared["qgc"] = np.stack([np.tile(f(inp["q_norm_g"])[l], 2)[:, None] for l in range(DEPTH)])
    shared["kgc"] = np.stack([np.tile(f(inp["k_norm_g"])[l], 2)[:, None] for l in range(DEPTH)])
    rpb = f(inp["na_rpb"])
    rp = np.zeros((DEPTH, 8, 17, 127), np.float32)
    rp[:, :, 1:16, 48:79] = rpb[:, :, :, ::-1]
    shared["rpbP"] = rp
    cw = f(inp["conv_w"])
    shared["cwT"] = np.ascontiguousarray(cw.reshape(DEPTH, 31, 4, 128).transpose(0, 3, 2, 1))
    shared["cbT"] = np.stack([_col(f(inp["conv_b"])[l], 4) for l in range(DEPTH)])
    shared["cgT"] = np.stack([_col(f(inp["conv_norm_g"])[l], 4) for l in range(DEPTH)])
    shared["w_branch"] = f(inp["w_branch"])
    shared["w_out"] = f(inp["w_out"])
    shared["wr"] = np.ascontiguousarray(f(inp["w_router"]).reshape(8, 128, NE).transpose(1, 0, 2))
    shared["rb"] = np.ascontiguousarray(np.broadcast_to(f(inp["router_bias"])[None, None, :], (128, 12, NE)))
    shared["w_gu"] = f(inp["w_expert_gu"])
    shared["w_dn"] = f(inp["w_expert_down"])
    shared["fng"] = np.ascontiguousarray(np.broadcast_to(f(inp["final_norm_g"])[None, :], (128, D)))
    x = f(inp["x"])
    c = f(inp["c"])
    ctx = f(inp["ctx"])
    cc = f(inp["c_ctx"])
    maps = []
    for core in range(8):
        b = core % 4
        m = dict(shared)
        m["x"] = x[b]
        m["ctx"] = ctx[b]
        m["cvec"] = np.ascontiguousarray(np.stack([_col(c[b], 8), _col(cc, 8)], axis=-1))
        half = core // 4
        m["sel"] = np.ascontiguousarray(np.broadcast_to(np.array([1.0 - half, float(half)], np.float32)[None, :], (128, 2)))
        maps.append(m)
    return maps


_NC_CACHE = {}


def kernel(**inputs):
    maps = _prep_inputs(inputs)
    if "nc" not in _NC_CACHE:
        _NC_CACHE["nc"] = build()
    nc = _NC_CACHE["nc"]
    res = run_bass_kernel_spmd(nc, maps, core_ids=list(range(8)))
    out = np.empty((4, S, D), np.float32)
    for core in range(8):
        b, half = core % 4, core // 4
        out[b, half * (S // 2):(half + 1) * (S // 2)] = np.asarray(res.results[core]["out"], dtype=np.float32)
    return out
```

```python
import contextlib
import numpy as np
import concourse.bass as bass
import concourse.mybir as mybir
from concourse.bass_utils import run_bass_kernel_spmd

F32 = mybir.dt.float32
BF16 = mybir.dt.bfloat16
AF = mybir.ActivationFunctionType
ALU = mybir.AluOpType
AX = mybir.AxisListType

D = 1024
S = 4096
C = 256
T = S + C
NT = T // 128
DEPTH = 2
INW = 7936
EPS = 1e-6
NE = 16
BIG = 1.0e9

DEBUG = False
STOP_AFTER = None
NLAYERS = DEPTH


class Tile:
    __slots__ = ("ap", "lw", "rd", "name")

    def __init__(self, ap, name=""):
        self.ap = ap
        self.lw = None
        self.rd = {}
        self.name = name

    def __getitem__(self, idx):
        return self.ap[idx]


class Sched:
    def __init__(self, nc, st):
        self.nc = nc
        self.eng = {"pe": nc.tensor, "act": nc.scalar, "dve": nc.vector, "pool": nc.gpsimd, "sp": nc.sync}
        self.st = st
        self.epoch = 0
        self.semobjs = {}
        self.sem = {}
        self.ekey = {}
        self.cnt = {}
        self.seen = {k: {} for k in self.eng}
        self._new_epoch()
        self.ND = 40
        self.dsem = [st.enter_context(nc.semaphore("dsem%d" % i)) for i in range(self.ND)]
        self.dcnt = [0] * self.ND
        self.dnext = 0
        self.NDH = 28
        self.dnext_sw = 0

    def _new_epoch(self):
        self.epoch += 1
        for k in self.eng:
            key = (k, self.epoch)
            self.semobjs[key] = self.st.enter_context(self.nc.semaphore("sem_%s_%d" % (k, self.epoch)))
            self.sem[k] = self.semobjs[key]
            self.ekey[k] = key
            self.cnt[k] = 0

    def _semobj(self, key):
        return self.dsem[key[1]] if key[0] == "d" else self.semobjs[key]

    def _wait(self, e, deps):
        best = {}
        for key, v in deps:
            if key[0] == "pe" and e == "pe":
                continue
            if v > best.get(key, 0):
                best[key] = v
        for key, v in best.items():
            if self.seen[e].get(key, 0) >= v:
                continue
            self.eng[e].wait_ge(self._semobj(key), v)
            self.seen[e][key] = v

    @staticmethod
    def _deps(r, w):
        deps = []
        for t in r:
            if t.lw is not None:
                deps.append(t.lw)
        for t in w:
            if t.lw is not None:
                deps.append(t.lw)
            deps.extend(t.rd.items())
        return deps

    @staticmethod
    def _stamp(r, w, key, v):
        for t in w:
            t.lw = (key, v)
            t.rd = {}
        for t in r:
            if t.rd.get(key, 0) < v:
                t.rd[key] = v

    def op(self, e, fn, r=(), w=()):
        self._wait(e, self._deps(r, w))
        ins = fn(self.eng[e])
        self.cnt[e] += 1
        ins.then_inc(self.sem[e], 1)
        self._stamp(r, w, self.ekey[e], self.cnt[e])

    def dma(self, q, out, in_, r=(), w=()):
        if q == "pool":
            i = self.NDH + self.dnext_sw
            self.dnext_sw = (self.dnext_sw + 1) % (self.ND - self.NDH)
        else:
            i = self.dnext
            self.dnext = (i + 1) % self.NDH
        deps = self._deps(r, w)
        if self.dcnt[i] > 0:
            deps.append((("d", i), self.dcnt[i]))
        self._wait(q, deps)
        self.eng[q].dma_start(out=out, in_=in_).then_inc(self.dsem[i], 16)
        self.dcnt[i] += 16
        self._stamp(r, w, ("d", i), self.dcnt[i])

    def barrier(self, new_epoch=False):
        deps = [(self.ekey[k], self.cnt[k]) for k in self.eng if self.cnt[k] > 0]
        deps += [(("d", i), self.dcnt[i]) for i in range(self.ND) if self.dcnt[i] > 0]
        for e in self.eng:
            for key, v in deps:
                if self.seen[e].get(key, 0) >= v:
                    continue
                self.eng[e].wait_ge(self._semobj(key), v)
                self.seen[e][key] = v
        if new_epoch:
            self._new_epoch()


class Ring:
    def __init__(self, tiles):
        self.tiles = tiles
        self.i = 0

    def next(self):
        t = self.tiles[self.i % len(self.tiles)]
        self.i += 1
        return t


def build(nlayers=NLAYERS):
    nc = bass.Bass("TRN2", target_bir_lowering=False)
    st = contextlib.ExitStack()
    with st:
        _build(nc, st, nlayers)
    return nc


def _build(nc, st, nlayers):
    sc = Sched(nc, st)
    op, dma = sc.op, sc.dma

    def din(name, shape):
        return nc.dram_tensor(name, list(shape), F32, kind="ExternalInput").ap()

    SK = "ExternalOutput" if DEBUG else "Internal"

    def dscr(name, shape, dt):
        return Tile(nc.dram_tensor(name, list(shape), dt, kind=SK).ap(), name)

    x_in = din("x", [S, D])
    ctx_in = din("ctx", [C, D])
    cvec_in = din("cvec", [128, 8, 2])
    w_mod = din("w_mod", [DEPTH, D, 6 * D])
    bmodT = din("bmodT", [DEPTH, 128, 48])
    n1g = din("n1g", [DEPTH, 128, 8])
    n2g = din("n2g", [DEPTH, 128, 8])
    w_in = din("w_in", [DEPTH, D, INW])
    qgc = din("qgc", [DEPTH, 128, 1])
    kgc = din("kgc", [DEPTH, 128, 1])
    rpbP = din("rpbP", [DEPTH, 8, 17, 127])
    cwT = din("cwT", [DEPTH, 128, 4, 31])
    cbT = din("cbT", [DEPTH, 128, 4])
    cgT = din("cgT", [DEPTH, 128, 4])
    w_branch = din("w_branch", [DEPTH, 4, 512, D])
    w_out = din("w_out", [DEPTH, D, D])
    wr_in = din("wr", [128, 8, NE])
    rb_in = din("rb", [128, 12, NE])
    SMALL_E = DEBUG and STOP_AFTER is not None and STOP_AFTER[1] != "E"
    w_gu = din("w_gu", [DEPTH, NE, D, D] if not SMALL_E else [1, 1, 128, 128])
    w_dn = din("w_dn", [DEPTH, NE, 512, D] if not SMALL_E else [1, 1, 128, 128])
    fng = din("fng", [128, D])
    k_c128 = din("c128", [128, 6 * 128])
    k_rope = din("rope", [128, 2, S])
    k_namask = din("namask", [128, 16 * 64])
    k_dft256 = din("dft256", [128, 2, 2, 256])
    k_f1 = din("f1", [64, 128])
    k_m12 = din("m12", [64, 2, 64 * 128])
    sel_in = din("sel", [128, 2])
    out_d = nc.dram_tensor("out", [S // 2, D], F32, kind="ExternalOutput").ap()

    X = dscr("X", [T, D], F32)
    X2 = dscr("X2", [S // 2, D], F32)
    QG = dscr("QG", [512, T], BF16)
    KG = dscr("KG", [256, T], BF16)
    VG = dscr("VG", [T, 2 * 128], BF16)
    QN = dscr("QN", [512, T], BF16)
    KN = dscr("KN", [512, T], BF16)
    VN = dscr("VN", [T, 8 * 128], BF16)
    U = dscr("U", [T, 512], BF16)
    ZC = dscr("ZC", [512, T], BF16)
    SG = dscr("SG", [4096, T], BF16)
    Y = dscr("Y", [2048, T], BF16)
    XIN = Tile(None, "xin")

    uid = [0]

    def dbg(name, tile, shape, dt=F32):
        if not DEBUG:
            return
        d_ = nc.dram_tensor("dbg_" + name, list(shape), dt, kind="ExternalOutput").ap()
        dma("sp", d_, tile[:], r=[tile], w=[Tile(None)])

    def sb(name, shape, dt=BF16, stack=st):
        uid[0] += 1
        return Tile(stack.enter_context(nc.sbuf_tensor("s%d_%s" % (uid[0], name), list(shape), dt)), name)

    def ring(name, n, shape, dt=BF16, stack=st):
        return Ring([sb("%s%d" % (name, i), shape, dt, stack) for i in range(n)])

    PB = [Tile(st.enter_context(nc.psum_tensor("pb%d" % i, [128, 512], F32)), "pb%d" % i) for i in range(7)]
    ptn = [0]

    def alloc_pt(stack):
        ptn[0] += 1
        return Tile(stack.enter_context(nc.psum_tensor("pt%d" % ptn[0], [128, 1024], BF16)), "pt")

    def alloc_pb7(stack):
        ptn[0] += 1
        return Tile(stack.enter_context(nc.psum_tensor("pb7_%d" % ptn[0], [128, 512], F32)), "pb7")

    c128 = sb("c128", [128, 6, 128])
    identf = sb("identf", [128, 128], F32)
    onesf = sb("onesf", [128, 128], F32)
    cvec = sb("cvec", [128, 8, 2], F32)
    scv = sb("scv", [128, 8, 2])
    wr = sb("wr", [128, 8, NE])
    rbt = sb("rbt", [128, 12, NE], F32)
    fngt = sb("fngt", [128, D], F32)
    selt = sb("selt", [128, 2], F32)
    dma("pool", c128[:], k_c128.rearrange("p (a b) -> p a b", b=128), w=[c128])
    dma("sp", identf[:], k_c128[:, 0:128], w=[identf])
    dma("sp", onesf[:], k_c128[:, 128:256], w=[onesf])
    dma("sp", cvec[:], cvec_in, w=[cvec])
    dma("pool", wr[:], wr_in, w=[wr])
    dma("sp", rbt[:], rb_in, w=[rbt])
    dma("sp", fngt[:], fng, w=[fngt])
    dma("sp", selt[:], sel_in, w=[selt])

    def blend(ta, apa, tb, apb):
        op("dve", lambda e: e.tensor_scalar(out=apb, in0=apb, scalar1=selt[:, 1:2], scalar2=None, op0=ALU.mult),
           r=[tb, selt], w=[tb])
        op("dve", lambda e: e.scalar_tensor_tensor(out=apa, in0=apa, scalar=selt[:, 0:1], in1=apb, op0=ALU.mult, op1=ALU.add),
           r=[ta, tb, selt], w=[ta])
    op("act", lambda e: e.activation(out=scv[:], in_=cvec[:], func=AF.Silu), r=[cvec], w=[scv])
    ident = c128[:, 0, :]
    ones = c128[:, 1, :]
    bdones = c128[:, 2, :]
    Pm = c128[:, 3, :]
    CCm = c128[:, 4, :]
    SCm = c128[:, 5, :]

    mod = sb("mod", [128, 48, 2], F32)
    sc1 = sb("sc1", [128, 8, 2], F32)
    sc2 = sb("sc2", [128, 8, 2], F32)
    gbc = [[sb("gbc%d%d" % (a, b), [128, D], F32) for b in range(2)] for a in range(2)]
    bmt = sb("bmt", [128, 48], F32)
    n1t = sb("n1t", [128, 8], F32)
    n2t = sb("n2t", [128, 8], F32)
    qgt = sb("qgt", [128, 1], F32)
    kgt = sb("kgt", [128, 1], F32)
    cwt = sb("cwt", [128, 4, 31], F32)
    cbt = sb("cbt", [128, 4], F32)
    cgt = sb("cgt", [128, 4], F32)

    def xsrc(l, t):
        if l == 0:
            if t < 32:
                return x_in[t * 128:(t + 1) * 128, :], XIN
            return ctx_in[(t - 32) * 128:(t - 31) * 128, :], XIN
        return X[t * 128:(t + 1) * 128, :], X

    def done(l, ph):
        return STOP_AFTER is not None and (l, ph) == STOP_AFTER

    def norm_tile(ps, xt, hT, col0, scl, shf_chunk0, i, xn_ring, ss_ring, trk=None):
        trk = hT if trk is None else trk
        junk = xn_ring.next()
        ss = ss_ring.next()
        op("act", lambda e: e.activation(out=junk[:], in_=xt[:], func=AF.Square, accum_out=ss[:, 0:1]),
           r=[xt], w=[junk, ss])
        op("act", lambda e: e.activation(out=ss[:, 1:2], in_=ss[:, 0:1], func=AF.Sqrt, scale=1.0 / D, bias=epsc[:, 0:1]),
           r=[ss, epsc], w=[ss])
        op("dve", lambda e: e.reciprocal(out=ss[:, 2:3], in_=ss[:, 1:2]), r=[ss], w=[ss])
        xn = xn_ring.next()
        op("dve", lambda e: e.tensor_scalar(out=xn[:], in0=xt[:], scalar1=ss[:, 2:3], scalar2=None, op0=ALU.mult),
           r=[xt, ss], w=[xn])
        for k in range(8):
            op("pe", lambda e: e.transpose(out=ps[:, k * 128:(k + 1) * 128], in_=xn[:, k * 128:(k + 1) * 128], identity=ident),
               r=[xn, c128], w=[ps])
        for k in range(8):
            eng = "act" if k % 2 == 0 else "dve"
            if eng == "act":
                op("act", lambda e: e.activation(out=hT[:, k, col0:col0 + 128], in_=ps[:, k * 128:(k + 1) * 128],
                                                 func=AF.Identity, scale=scl[:, k, i:i + 1],
                                                 bias=mod[:, shf_chunk0 + k, i:i + 1]),
                   r=[ps, scl, mod], w=[trk])
            else:
                op("dve", lambda e: e.tensor_scalar(out=hT[:, k, col0:col0 + 128], in0=ps[:, k * 128:(k + 1) * 128],
                                                    scalar1=scl[:, k, i:i + 1], scalar2=mod[:, shf_chunk0 + k, i:i + 1],
                                                    op0=ALU.mult, op1=ALU.add),
                   r=[ps, scl, mod], w=[trk])

    epsc = sb("epsc", [128, 1], F32)
    op("dve", lambda e: e.memset(epsc[:], EPS), w=[epsc])

    for l in range(nlayers):
        last = (l == DEPTH - 1)
        upd_ctx = not last
        ntl = NT if upd_ctx else 32

        sc.barrier(new_epoch=(l > 0))
        with contextlib.ExitStack() as ps_:
            wm = ring("wm", 2, [128, 8, 1536], BF16, ps_)
            dgf = ring("dgf", 2, [128, 128], F32, ps_)
            for t_, src in ((bmt, bmodT[l]), (n1t, n1g[l]), (n2t, n2g[l]), (qgt, qgc[l]), (kgt, kgc[l]),
                            (cwt, cwT[l]), (cbt, cbT[l]), (cgt, cgT[l])):
                dma("sp", t_[:], src, w=[t_])
            psM = PB[0]
            for cb in range(4):
                w_ = wm.next()
                dma("pool", w_[:], w_mod[l][:, cb * 1536:(cb + 1) * 1536].rearrange("(k p) n -> p k n", p=128), w=[w_])
                for j in range(12):
                    ch = cb * 12 + j
                    for k in range(8):
                        op("pe", lambda e: e.matmul(psM[:, ch * 2:ch * 2 + 2], lhsT=w_[:, k, j * 128:(j + 1) * 128],
                                                    rhs=scv[:, k, :], start=(k == 0), stop=(k == 7)),
                           r=[w_, scv], w=[psM])
            for i in range(2):
                op("dve", lambda e: e.tensor_tensor(out=mod[:, :, i], in0=psM[:, i:96:2], in1=bmt[:], op=ALU.add),
                   r=[psM, bmt], w=[mod])
            for (sct, c0, nt_) in ((sc1, 8, n1t), (sc2, 32, n2t)):
                for i in range(2):
                    op("dve", lambda e: e.scalar_tensor_tensor(out=sct[:, :, i], in0=mod[:, c0:c0 + 8, i], scalar=1.0,
                                                               in1=nt_[:], op0=ALU.add, op1=ALU.mult),
                       r=[mod, nt_], w=[sct])
            for a, c0 in ((0, 16), (1, 40)):
                for i in range(2):
                    for hf in range(2):
                        pg = PB[1 + hf]
                        for kk in range(4):
                            k = hf * 4 + kk
                            dg = dgf.next()
                            op("dve", lambda e: e.tensor_scalar(out=dg[:], in0=identf[:], scalar1=mod[:, c0 + k, i:i + 1],
                                                                scalar2=None, op0=ALU.mult), r=[identf, mod], w=[dg])
                            op("pe", lambda e: e.matmul(pg[:, kk * 128:(kk + 1) * 128], lhsT=onesf[:], rhs=dg[:],
                                                        start=True, stop=True), r=[onesf, dg], w=[pg])
                        op("act", lambda e: e.activation(out=gbc[a][i][:, hf * 512:(hf + 1) * 512], in_=pg[:], func=AF.Copy),
                           r=[pg], w=[gbc[a][i]])
        if l == 0:
            dbg("mod", mod, [128, 48, 2])
            dbg("sc1", sc1, [128, 8, 2])
            dbg("g1lat", gbc[0][0], [128, D])
            dbg("g2ctx", gbc[1][1], [128, D])
        if done(l, "M"):
            break

        sc.barrier()
        with contextlib.ExitStack() as ps_:
            PT = alloc_pt(ps_)
            hT = sb("hT", [128, 8, T], BF16, ps_)
            hTs = [Tile(hT.ap, "hT%d" % b_) for b_ in range(9)]
            rope = sb("rope", [128, 2, S], BF16, ps_)
            dma("pool", rope[:], k_rope, w=[rope])
            xr = ring("xr", 2, [128, D], F32, ps_)
            xnr = ring("xnr", 3, [128, D], BF16, ps_)
            ssr = ring("ssr", 3, [128, 4], F32, ps_)
            for t in range(NT):
                xt = xr.next()
                src, srct = xsrc(l, t)
                dma("sp", xt[:], src, r=[srct], w=[xt])
                norm_tile(PT, xt, hT, t * 128, sc1, 0, 0 if t < 32 else 1, xnr, ssr, trk=hTs[t // 4])

            wbuf = ring("wbuf", 2, [128, 8, 512], BF16, ps_)
            ob = ring("ob", 3, [128, 512], BF16, ps_)
            obv = ring("obv", 2, [128, 8, 128], BF16, ps_)
            sqb = ring("sqb", 2, [128, 512], BF16, ps_)
            rsf = ring("rsf", 2, [128, 512], F32, ps_)
            qnb = ring("qnb", 2, [128, 512], BF16, ps_)
            t1r = ring("t1r", 2, [128, 512], F32, ps_)
            t2r = ring("t2r", 2, [128, 512], F32, ps_)
            sgr = ring("sgr", 2, [128, 512], F32, ps_)
            for t_ in obv.tiles:
                op("dve", lambda e: e.memset(t_[:], 1.0), w=[t_])
            pmain = Ring(PB[0:4])
            blocks = [(b * 512, 512) for b in range(8)] + [(S, C)]

            def load_w(c0, n):
                w_ = wbuf.next()
                dma("pool", w_[:, :, 0:n], w_in[l][:, c0:c0 + n].rearrange("(k p) n -> p k n", p=128), w=[w_])
                return w_

            def mm_fm(ps, w_, wc0, t0, nt):
                for k in range(8):
                    op("pe", lambda e: e.matmul(ps[:, 0:nt], lhsT=w_[:, k, wc0:wc0 + 128], rhs=hT[:, k, t0:t0 + nt],
                                                start=(k == 0), stop=(k == 7)), r=[w_, hTs[t0 // 512]], w=[ps])

            def qk_prep(ps, nt, gcol, t0, dst, row0):
                is_lat = t0 < S
                sq = sqb.next()
                op("act", lambda e: e.activation(out=sq[:, 0:nt], in_=ps[:, 0:nt], func=AF.Square), r=[ps], w=[sq])
                pa = PB[4]
                op("pe", lambda e: e.matmul(pa[:, 0:nt], lhsT=bdones, rhs=sq[:, 0:nt], start=True, stop=True),
                   r=[c128, sq], w=[pa])
                rs = rsf.next()
                op("act", lambda e: e.activation(out=rs[:, 0:nt], in_=pa[:, 0:nt], func=AF.Sqrt, scale=1.0 / 64,
                                                 bias=epsc[:, 0:1]), r=[pa, epsc], w=[rs])
                op("dve", lambda e: e.reciprocal(out=rs[:, 0:nt], in_=rs[:, 0:nt]), r=[rs], w=[rs])
                qn = qnb.next()
                op("dve", lambda e: e.scalar_tensor_tensor(out=qn[:, 0:nt], in0=ps[:, 0:nt], scalar=gcol[:, 0:1],
                                                           in1=rs[:, 0:nt], op0=ALU.mult, op1=ALU.mult),
                   r=[ps, gcol, rs], w=[qn])
                if is_lat:
                    pb_ = PB[5]
                    op("pe", lambda e: e.matmul(pb_[:, 0:nt], lhsT=Pm, rhs=qn[:, 0:nt], start=True, stop=True),
                       r=[c128, qn], w=[pb_])
                    t1 = t1r.next()
                    t2 = t2r.next()
                    op("pool", lambda e: e.tensor_tensor(out=t1[:, 0:nt], in0=qn[:, 0:nt], in1=rope[:, 0, t0:t0 + nt],
                                                         op=ALU.mult), r=[qn, rope], w=[t1])
                    op("dve", lambda e: e.tensor_tensor(out=t2[:, 0:nt], in0=pb_[:, 0:nt], in1=rope[:, 1, t0:t0 + nt],
                                                        op=ALU.mult), r=[pb_, rope], w=[t2])
                    o = ob.next()
                    op("pool", lambda e: e.tensor_tensor(out=o[:, 0:nt], in0=t1[:, 0:nt], in1=t2[:, 0:nt], op=ALU.add),
                       r=[t1, t2], w=[o])
                    dma("sp", dst[row0:row0 + 128, t0:t0 + nt], o[:, 0:nt], r=[o], w=[dst])
                else:
                    dma("sp", dst[row0:row0 + 128, t0:t0 + nt], qn[:, 0:nt], r=[qn], w=[dst])

            w_ = load_w(0, 512)
            units = [(t0, nt, cch) for (t0, nt) in blocks for cch in range(4)]
            pend_ = None
            for (t0, nt, cch) in units:
                ps = pmain.next()
                mm_fm(ps, w_, cch * 128, t0, nt)
                if pend_ is not None:
                    qk_prep(*pend_)
                pend_ = (ps, nt, qgt, t0, QG, cch * 128)
            qk_prep(*pend_)
            w_ = wbuf.next()
            for h in range(2):
                for dup in range(2):
                    dma("pool", w_[:, :, h * 128 + dup * 64:h * 128 + dup * 64 + 64],
                        w_in[l][:, 512 + h * 64:512 + (h + 1) * 64].rearrange("(k p) n -> p k n", p=128), w=[w_])
            units = [(t0, nt, h) for (t0, nt) in blocks for h in range(2)]
            pend_ = None
            for (t0, nt, h) in units:
                ps = pmain.next()
                mm_fm(ps, w_, h * 128, t0, nt)
                if pend_ is not None:
                    qk_prep(*pend_)
                pend_ = (ps, nt, kgt, t0, KG, h * 128)
            qk_prep(*pend_)

            def mm_tm(ps, w_, n, t):
                for k in range(8):
                    op("pe", lambda e: e.matmul(ps[:, 0:n], lhsT=hT[:, k, t * 128:(t + 1) * 128], rhs=w_[:, k, 0:n],
                                                start=(k == 0), stop=(k == 7)), r=[w_, hTs[t // 4]], w=[ps])

            w_ = load_w(640, 128)
            for t in range(NT):
                ps = pmain.next()
                mm_tm(ps, w_, 128, t)
                o = obv.next()
                op("dve", lambda e: e.tensor_copy(out=o[:, 0:2, 0:64], in_=ps[:, 0:128].rearrange("p (h d) -> p h d", d=64)),
                   r=[ps], w=[o])
                dma("sp", VG[t * 128:(t + 1) * 128, :].rearrange("p (h d) -> p h d", d=128), o[:, 0:2, :], r=[o], w=[VG])
            for (c0, dst) in ((768, QN), (1280, KN)):
                w_ = load_w(c0, 512)
                for (t0, nt) in blocks:
                    for cch in range(4):
                        ps = pmain.next()
                        mm_fm(ps, w_, cch * 128, t0, nt)
                        o = ob.next()
                        op("act", lambda e: e.activation(out=o[:, 0:nt], in_=ps[:, 0:nt], func=AF.Copy), r=[ps], w=[o])
                        dma("sp", dst[cch * 128:(cch + 1) * 128, t0:t0 + nt], o[:, 0:nt], r=[o], w=[dst])
            w_ = load_w(1792, 512)
            for t in range(NT):
                ps = pmain.next()
                mm_tm(ps, w_, 512, t)
                o = obv.next()
                op("dve", lambda e: e.tensor_copy(out=o[:, :, 0:64], in_=ps[:, :].rearrange("p (h d) -> p h d", d=64)),
                   r=[ps], w=[o])
                dma("sp", VN[t * 128:(t + 1) * 128, :].rearrange("p (h d) -> p h d", d=128), o[:], r=[o], w=[VN])
            w_ = load_w(2304, 512)
            for t in range(NT):
                ps = pmain.next()
                mm_tm(ps, w_, 512, t)
                o = ob.next()
                op("act", lambda e: e.activation(out=o[:], in_=ps[:], func=AF.Copy), r=[ps], w=[o])
                dma("sp", U[t * 128:(t + 1) * 128, :], o[:], r=[o], w=[U])
            wa = load_w(2816, 512)
            wb_ = load_w(3328, 512)
            for (t0, nt) in blocks:
                for cch in range(4):
                    pa_ = pmain.next()
                    pb_ = pmain.next()
                    mm_fm(pa_, wa, cch * 128, t0, nt)
                    mm_fm(pb_, wb_, cch * 128, t0, nt)
                    sg_ = sgr.next()
                    op("act", lambda e: e.activation(out=sg_[:, 0:nt], in_=pb_[:, 0:nt], func=AF.Sigmoid), r=[pb_], w=[sg_])
                    o = ob.next()
                    op("dve", lambda e: e.tensor_tensor(out=o[:, 0:nt], in0=pa_[:, 0:nt], in1=sg_[:, 0:nt], op=ALU.mult),
                       r=[pa_, sg_], w=[o])
                    dma("sp", ZC[cch * 128:(cch + 1) * 128, t0:t0 + nt], o[:, 0:nt], r=[o], w=[ZC])
            nblk = len(blocks) if upd_ctx else 8
            for gq_ in range(8):
                w_ = load_w(3840 + gq_ * 512, 512)
                for (t0, nt) in blocks[:nblk]:
                    for cch in range(4):
                        ps = pmain.next()
                        mm_fm(ps, w_, cch * 128, t0, nt)
                        o = ob.next()
                        op("act", lambda e: e.activation(out=o[:, 0:nt], in_=ps[:, 0:nt], func=AF.Sigmoid), r=[ps], w=[o])
                        row0 = (gq_ * 4 + cch) * 128
                        dma("sp", SG[row0:row0 + 128, t0:t0 + nt], o[:, 0:nt], r=[o], w=[SG])
        if done(l, "P"):
            break

        def dense_heads(heads, nt, chunks, ptr, rtr, rdeps):
            n = len(chunks)

            def s_step(hd, jj):
                ps = hd["pS"][jj % len(hd["pS"])]
                op("pe", lambda e: e.matmul(ps[:, 0:nt], lhsT=hd["kfn"](chunks[jj]), rhs=hd["qap"], start=True, stop=True),
                   r=rdeps, w=[ps])
                return ps
            LK = min(len(heads[0]["pS"]) - 1, n)
            pend = [[s_step(hd, jj) for hd in heads] for jj in range(LK)]
            for jj in range(n):
                if jj + LK < n:
                    pend.append([s_step(hd, jj + LK) for hd in heads])
                cur = pend.pop(0)
                for hi, hd in enumerate(heads):
                    pt = ptr.next()
                    c_ = cur[hi]
                    op("act", lambda e: e.activation(out=pt[:, 0:nt], in_=c_[:, 0:nt], func=AF.Exp, scale=0.125),
                       r=[c_], w=[pt])
                    op("pe", lambda e: e.matmul(hd["acc"][:, 0:nt], lhsT=hd["vfn"](chunks[jj]), rhs=pt[:, 0:nt],
                                                start=(jj == 0), stop=(jj == n - 1)), r=rdeps + [pt], w=[hd["acc"]])
            for hd in heads:
                rt = rtr.next()
                acc = hd["acc"]
                op("dve", lambda e: e.reciprocal(out=rt[0:64, 0:nt], in_=acc[64:128, 0:nt]), r=[acc], w=[rt])
                op("dve", lambda e: e.tensor_tensor(out=hd["yap"], in0=acc[0:64, 0:nt], in1=rt[0:64, 0:nt],
                                                    op=ALU.mult), r=[acc, rt], w=[hd["ytile"]])

        sc.barrier()
        with contextlib.ExitStack() as ps_:
            PB7 = alloc_pb7(ps_)
            kgT = sb("kgT", [128, 2, T], BF16, ps_)
            vga = sb("vga", [128, NT, 2, 128], BF16, ps_)
            dma("sp", kgT[:], KG[:, :].rearrange("(h p) t -> p h t", p=128), r=[KG], w=[kgT])
            dma("sp", vga[:], VG[:, :].rearrange("(j p) (h d) -> p j h d", p=128, d=128), r=[VG], w=[vga])
            qtr = ring("qtr", 2, [128, 512], BF16, ps_)
            ptr = ring("ptr", 6, [128, 512], BF16, ps_)
            rtr = ring("rtr", 2, [64, 512], F32, ps_)
            ytr = ring("ytr", 4, [64, 512], BF16, ps_)
            accr = Ring([PB[6], PB7])
            blocks = [(b * 512, 512, list(range(NT))) for b in range(4 if last else 8)]
            if upd_ctx:
                blocks.append((S, C, [32, 33]))
            qt2r = ring("qt2r", 2, [128, 512], BF16, ps_)
            for (t0, nt, chunks) in blocks:
                for cch in range(4):
                    qt = qtr.next()
                    dma("sp", qt[:, 0:nt], QG[cch * 128:(cch + 1) * 128, t0:t0 + nt], r=[QG], w=[qt])
                    if last:
                        q2 = qt2r.next()
                        dma("sp", q2[:, 0:nt], QG[cch * 128:(cch + 1) * 128, t0 + S // 2:t0 + S // 2 + nt], r=[QG], w=[q2])
                        blend(qt, qt[:, 0:nt], q2, q2[:, 0:nt])
                    heads = []
                    for hp in range(2):
                        hq = cch * 2 + hp
                        kvh = hq // 4
                        P0 = hp * 64
                        yt = ytr.next()
                        heads.append(dict(qap=qt[P0:P0 + 64, 0:nt],
                                          kfn=(lambda j, P0=P0, kvh=kvh: kgT[P0:P0 + 64, kvh, j * 128:(j + 1) * 128]),
                                          vfn=(lambda j, kvh=kvh: vga[:, j, kvh, :]),
                                          pS=PB[3 * hp:3 * hp + 3], acc=accr.next(), ytile=yt, yap=yt[0:64, 0:nt], hq=hq))
                    dense_heads(heads, nt, chunks, ptr, rtr, [kgT, vga, qt])
                    for hd in heads:
                        hq = hd["hq"]
                        dma("sp", Y[hq * 64:(hq + 1) * 64, t0:t0 + nt], hd["ytile"][0:64, 0:nt], r=[hd["ytile"]], w=[Y])
                        if last:
                            dma("sp", Y[hq * 64:(hq + 1) * 64, t0 + S // 2:t0 + S // 2 + nt], hd["ytile"][0:64, 0:nt],
                                r=[hd["ytile"]], w=[Y])
        if done(l, "G"):
            break

        sc.barrier()
        with contextlib.ExitStack() as ps_:
            knT = sb("knT", [128, 4, T], BF16, ps_)
            vna = sb("vna", [128, NT, 8, 128], BF16, ps_)
            tst = sb("tst", [128, 8, 16, 64], BF16, ps_)
            dma("sp", knT[:], KN[:, :].rearrange("(c p) t -> p c t", p=128), r=[KN], w=[knT])
            dma("sp", vna[:], VN[:, :].rearrange("(j p) (h d) -> p j h d", p=128, d=128), r=[VN], w=[vna])
            namask = sb("namask", [128, 16, 64], BF16, ps_)
            dma("pool", namask[:], k_namask.rearrange("p (a b) -> p a b", b=64), w=[namask])
            rawt = [Tile(None, "raw%d" % p) for p in range(128)]
            for p in range(128):
                half, kc = divmod(p, 64)
                dma("pool", tst[p:p + 1, :, :, :], rpbP[l][:, half:half + 16, 63 - kc:63 - kc + 64], w=[rawt[p]])
            op("act", lambda e: e.activation(out=tst[:], in_=tst[:], func=AF.Exp), r=rawt, w=[tst])
            for h in range(8):
                op("dve", lambda e: e.tensor_tensor(out=tst[:, h], in0=tst[:, h], in1=namask[:], op=ALU.mult),
                   r=[tst, namask], w=[tst])
            qnr = ring("qnr", 2, [128, 4, 512], BF16, ps_)
            ptr = ring("ptr", 6, [128, 512], BF16, ps_)
            rtr = ring("rtr", 2, [64, 512], F32, ps_)
            ynr = ring("ynr", 2, [64, 8, 512], BF16, ps_)
            accr = Ring(PB[2:4])
            pS3 = [PB[0], PB[1], PB[4], PB[5]]
            LOOK = 3
            ucnt = [0]
            for b in range(8):
                qn = qnr.next()
                dma("sp", qn[:], QN[:, b * 512:(b + 1) * 512].rearrange("(c p) t -> p c t", p=128), r=[QN], w=[qn])
                yn = ynr.next()
                accs = [accr.next() for rl_ in range(8)]

                def na_S(rl, h, qn=qn, b=b):
                    r_ = b * 8 + rl
                    r0 = min(max(r_ - 4, 0), 56)
                    i0 = r0 // 2
                    nck = 4 if r0 % 2 == 0 else 5
                    chunks = [i0 + j for j in range(nck)] + [32, 33]
                    dri = 2 * i0 - r_ + 8
                    cch, P0 = h // 2, (h % 2) * 64
                    ps = pS3[ucnt[0] % len(pS3)]
                    ucnt[0] += 1
                    for jj, j in enumerate(chunks):
                        op("pe", lambda e: e.matmul(ps[:, jj * 64:(jj + 1) * 64], lhsT=knT[P0:P0 + 64, cch, j * 128:(j + 1) * 128],
                                                    rhs=qn[P0:P0 + 64, cch, rl * 64:(rl + 1) * 64], start=True, stop=True),
                           r=[knT, qn], w=[ps])
                    return (ps, rl, h, r0, nck, chunks, dri)

                def na_rest(u_, yn=yn, accs=accs):
                    ps, rl, h, r0, nck, chunks, dri = u_
                    ncol = (nck + 2) * 64
                    acc = accs[rl]
                    pt = ptr.next()
                    op("act", lambda e: e.activation(out=pt[:, 0:ncol], in_=ps[:, 0:ncol], func=AF.Exp, scale=0.125),
                       r=[ps], w=[pt])
                    op("pool", lambda e: e.tensor_tensor(out=pt[:, 0:nck * 64].rearrange("p (a b) -> p a b", b=64),
                                                         in0=pt[:, 0:nck * 64].rearrange("p (a b) -> p a b", b=64),
                                                         in1=tst[:, h, dri:dri + 2 * nck:2, :], op=ALU.mult),
                       r=[pt, tst], w=[pt])
                    if r0 % 2 == 1:
                        op("pool", lambda e: e.memset(pt[0:64, 0:64], 0.0), w=[pt])
                        op("pool", lambda e: e.memset(pt[64:128, (nck - 1) * 64:nck * 64], 0.0), w=[pt])
                    for jj, j in enumerate(chunks):
                        op("pe", lambda e: e.matmul(acc[:, h * 64:(h + 1) * 64], lhsT=vna[:, j, h, :],
                                                    rhs=pt[:, jj * 64:(jj + 1) * 64], start=(jj == 0),
                                                    stop=(jj == len(chunks) - 1)), r=[vna, pt], w=[acc])
                    if h == 7:
                        rt = rtr.next()
                        op("dve", lambda e: e.reciprocal(out=rt[0:64, :], in_=acc[64:128, :]), r=[acc], w=[rt])
                        op("dve", lambda e: e.tensor_tensor(out=yn[0:64, :, rl * 64:(rl + 1) * 64],
                                                            in0=acc[0:64, :].rearrange("p (h q) -> p h q", q=64),
                                                            in1=rt[0:64, :].rearrange("p (h q) -> p h q", q=64), op=ALU.mult),
                           r=[acc, rt], w=[yn])

                units = [(rl, h) for rl in range(8) for h in range(8)]
                pend = []
                for idx in range(len(units) + LOOK):
                    if idx < len(units):
                        pend.append(na_S(*units[idx]))
                    if idx >= LOOK:
                        na_rest(pend.pop(0))
                dma("sp", Y[512:1024, b * 512:(b + 1) * 512].rearrange("(h p) t -> p h t", p=64), yn[0:64, :, :], r=[yn], w=[Y])
            if upd_ctx:
                qn = qnr.next()
                dma("sp", qn[:, :, 0:C], QN[:, S:T].rearrange("(c p) t -> p c t", p=128), r=[QN], w=[qn])
                yn = ynr.next()
                acc3 = Ring([PB[6], PB[2], PB[3]])
                for cch in range(4):
                    heads = []
                    for hp in range(2):
                        h = cch * 2 + hp
                        P0 = hp * 64
                        heads.append(dict(qap=qn[P0:P0 + 64, cch, 0:C],
                                          kfn=(lambda j, P0=P0, cch=cch: knT[P0:P0 + 64, cch, j * 128:(j + 1) * 128]),
                                          vfn=(lambda j, h=h: vna[:, j, h, :]),
                                          pS=[PB[0], PB[1]] if hp == 0 else [PB[4], PB[5]], acc=acc3.next(), ytile=yn,
                                          yap=yn[0:64, h, 0:C]))
                    dense_heads(heads, C, [32, 33], ptr, rtr, [knT, vna, qn])
                dma("sp", Y[512:1024, S:T].rearrange("(h p) t -> p h t", p=64), yn[0:64, :, 0:C], r=[yn], w=[Y])
        if done(l, "A"):
            break

        sc.barrier()
        with contextlib.ExitStack() as ps_:
            dft256 = sb("dft256", [128, 2, 2, 256], BF16, ps_)
            f1 = sb("f1", [64, 128], BF16, ps_)
            m12 = sb("m12", [64, 2, 64, 128], BF16, ps_)
            dma("pool", dft256[:], k_dft256, w=[dft256])
            dma("pool", f1[:], k_f1, w=[f1])
            dma("pool", m12[:], k_m12.rearrange("p a (b c) -> p a b c", c=128), w=[m12])
            u1r = ring("u1r", 2, [64, 64, 128], BF16, ps_)
            a1 = sb("a1", [64, 128, 128], BF16, ps_)
            xt_ = sb("xtf", [128, 2, S], BF16, ps_)
            yo = ring("yo", 2, [128, 512], BF16, ps_)
            pr = Ring(PB[0:4])
            sclat = 1.0 / float(np.sqrt(S * 128.0))
            for g in range(4):
                u1 = u1r.next()
                dma("sp", u1[:], U[0:S, g * 128:(g + 1) * 128].rearrange("(a b) c -> a b c", b=64), r=[U], w=[u1])
                for c4 in range(32):
                    ps = pr.next()
                    for cc in range(4):
                        c = c4 * 4 + cc
                        op("pe", lambda e: e.matmul(ps[0:64, cc * 128:(cc + 1) * 128], lhsT=u1[:, :, c], rhs=f1[:],
                                                    start=True, stop=True), r=[u1, f1], w=[ps])
                    eng = "act" if c4 % 2 == 0 else "dve"
                    if eng == "act":
                        op("act", lambda e: e.activation(out=a1[:, c4 * 4:c4 * 4 + 4, :],
                                                         in_=ps[0:64, :].rearrange("p (a b) -> p a b", b=128), func=AF.Copy),
                           r=[ps], w=[a1])
                    else:
                        op("dve", lambda e: e.tensor_copy(out=a1[:, c4 * 4:c4 * 4 + 4, :],
                                                          in_=ps[0:64, :].rearrange("p (a b) -> p a b", b=128)),
                           r=[ps], w=[a1])
                for k4 in range(16):
                    ps = pr.next()
                    for kk in range(4):
                        k1 = k4 * 4 + kk
                        op("pe", lambda e: e.matmul(ps[:, kk * 128:(kk + 1) * 128], lhsT=a1[:, :, k1], rhs=m12[:, 0, k1, :],
                                                    start=True, stop=False), r=[a1, m12], w=[ps])
                        op("pe", lambda e: e.matmul(ps[:, kk * 128:(kk + 1) * 128], lhsT=a1[:, :, 64 + k1], rhs=m12[:, 1, k1, :],
                                                    start=False, stop=True), r=[a1, m12], w=[ps])
                    for ri in range(2):
                        src = ps[:, :].rearrange("p (a r k) -> p a r k", a=4, r=2)[:, :, ri, :]
                        dst = xt_[:, ri, :].rearrange("p (k a) -> p a k", a=64)[:, k4 * 4:k4 * 4 + 4, :]
                        if ri == 0:
                            op("act", lambda e: e.activation(out=dst, in_=src, func=AF.Copy), r=[ps], w=[xt_])
                        else:
                            op("dve", lambda e: e.tensor_copy(out=dst, in_=src), r=[ps], w=[xt_])
                for b in range(8):
                    ps = pr.next()
                    op("pe", lambda e: e.matmul(ps[:], lhsT=CCm, rhs=xt_[:, 0, b * 512:(b + 1) * 512], start=True, stop=False),
                       r=[c128, xt_], w=[ps])
                    op("pe", lambda e: e.matmul(ps[:], lhsT=SCm, rhs=xt_[:, 1, b * 512:(b + 1) * 512], start=False, stop=True),
                       r=[c128, xt_], w=[ps])
                    o = yo.next()
                    op("act", lambda e: e.activation(out=o[:], in_=ps[:], func=AF.Copy, scale=sclat), r=[ps], w=[o])
                    dma("sp", Y[1024 + g * 128:1024 + (g + 1) * 128, b * 512:(b + 1) * 512], o[:], r=[o], w=[Y])
            if upd_ctx:
                uc = sb("uc", [128, 2, 512], BF16, ps_)
                xc_ = sb("xcf", [128, 2, C], BF16, ps_)
                dma("sp", uc[:], U[S:T, :].rearrange("(n p) c -> p n c", p=128), r=[U], w=[uc])
                scc = 1.0 / float(np.sqrt(C * 128.0))
                for g in range(4):
                    ps = pr.next()
                    for ri in range(2):
                        for n in range(2):
                            op("pe", lambda e: e.matmul(ps[:, ri * C:(ri + 1) * C], lhsT=uc[:, n, g * 128:(g + 1) * 128],
                                                        rhs=dft256[:, ri, n, :], start=(n == 0), stop=(n == 1)),
                               r=[uc, dft256], w=[ps])
                    op("act", lambda e: e.activation(out=xc_[:], in_=ps[:, :].rearrange("p (r k) -> p r k", r=2), func=AF.Copy),
                       r=[ps], w=[xc_])
                    ps2 = pr.next()
                    op("pe", lambda e: e.matmul(ps2[:, 0:C], lhsT=CCm, rhs=xc_[:, 0, :], start=True, stop=False),
                       r=[c128, xc_], w=[ps2])
                    op("pe", lambda e: e.matmul(ps2[:, 0:C], lhsT=SCm, rhs=xc_[:, 1, :], start=False, stop=True),
                       r=[c128, xc_], w=[ps2])
                    o = yo.next()
                    op("act", lambda e: e.activation(out=o[:, 0:C], in_=ps2[:, 0:C], func=AF.Copy, scale=scc), r=[ps2], w=[o])
                    dma("sp", Y[1024 + g * 128:1024 + (g + 1) * 128, S:T], o[:, 0:C], r=[o], w=[Y])
        if done(l, "F"):
            break

        sc.barrier()
        with contextlib.ExitStack() as ps_:
            dg = sb("dg", [128, 4, 31, 128], BF16, ps_)
            for cch in range(4):
                for k in range(31):
                    op("dve", lambda e: e.tensor_scalar(out=dg[:, cch, k, :], in0=ident, scalar1=cwt[:, cch, k:k + 1],
                                                        scalar2=None, op0=ALU.mult), r=[c128, cwt], w=[dg])
            zc = sb("zc", [128, 4, S + 30], BF16, ps_)
            zcc = sb("zcc", [128, 4, C + 30], BF16, ps_)
            ycr = ring("ycr", 2, [128, 4, 512], F32, ps_)
            sqr = ring("sqr", 2, [128, 4, 512], BF16, ps_)
            rsr = ring("rsr", 2, [128, 512], F32, ps_)
            tmr = ring("tmr", 2, [128, 512], F32, ps_)
            yo = ring("yoc", 3, [128, 512], BF16, ps_)
            seqs = [(zc, 0, S)] + ([(zcc, S, C)] if upd_ctx else [])
            for (zt, s0, L) in seqs:
                op("dve", lambda e: e.memset(zt[:, :, 0:15], 0.0), w=[zt])
                op("dve", lambda e: e.memset(zt[:, :, 15 + L:30 + L], 0.0), w=[zt])
                dma("sp", zt[:, :, 15:15 + L], ZC[:, s0:s0 + L].rearrange("(c p) t -> p c t", p=128), r=[ZC], w=[zt])
                nt = min(512, L)
                for b in range(L // nt):
                    t0 = b * nt
                    yc = ycr.next()
                    sq = sqr.next()
                    for cch in range(4):
                        ps = PB[cch]
                        for k in range(31):
                            op("pe", lambda e: e.matmul(ps[:, 0:nt], lhsT=dg[:, cch, k, :], rhs=zt[:, cch, t0 + k:t0 + k + nt],
                                                        start=(k == 0), stop=(k == 30)), r=[dg, zt], w=[ps])
                        op("act", lambda e: e.activation(out=yc[:, cch, 0:nt], in_=ps[:, 0:nt], func=AF.Identity,
                                                         bias=cbt[:, cch:cch + 1]), r=[ps, cbt], w=[yc])
                        op("act", lambda e: e.activation(out=sq[:, cch, 0:nt], in_=ps[:, 0:nt], func=AF.Square,
                                                         bias=cbt[:, cch:cch + 1]), r=[ps, cbt], w=[sq])
                    pss = PB[4]
                    for cch in range(4):
                        op("pe", lambda e: e.matmul(pss[:, 0:nt], lhsT=ones, rhs=sq[:, cch, 0:nt], start=(cch == 0),
                                                    stop=(cch == 3)), r=[c128, sq], w=[pss])
                    rs = rsr.next()
                    op("act", lambda e: e.activation(out=rs[:, 0:nt], in_=pss[:, 0:nt], func=AF.Sqrt, scale=1.0 / 512,
                                                     bias=epsc[:, 0:1]), r=[pss, epsc], w=[rs])
                    op("dve", lambda e: e.reciprocal(out=rs[:, 0:nt], in_=rs[:, 0:nt]), r=[rs], w=[rs])
                    for cch in range(4):
                        tm = tmr.next()
                        op("dve", lambda e: e.tensor_tensor(out=tm[:, 0:nt], in0=yc[:, cch, 0:nt], in1=rs[:, 0:nt], op=ALU.mult),
                           r=[yc, rs], w=[tm])
                        o = yo.next()
                        op("act", lambda e: e.activation(out=o[:, 0:nt], in_=tm[:, 0:nt], func=AF.Silu,
                                                         scale=cgt[:, cch:cch + 1]), r=[tm, cgt], w=[o])
                        dma("sp", Y[1536 + cch * 128:1536 + (cch + 1) * 128, s0 + t0:s0 + t0 + nt], o[:, 0:nt], r=[o], w=[Y])
        if done(l, "C"):
            break

        sc.barrier()
        with contextlib.ExitStack() as ps_:
            wbr = sb("wbr", [128, 16, D], BF16, ps_)
            wo = sb("wo", [128, 8, D], BF16, ps_)
            for n in range(4):
                dma("pool", wbr[:, n * 4:(n + 1) * 4, :], w_branch[l, n].rearrange("(k p) d -> p k d", p=128), w=[wbr])
            dma("pool", wo[:], w_out[l].rearrange("(k p) d -> p k d", p=128), w=[wo])
            ybr = ring("ybr", 2, [128, 16, 512], BF16, ps_)
            sgr = ring("sgb", 3, [128, 4, 512], BF16, ps_)
            accf = ring("accf", 2, [128, 512], F32, ps_)
            tmpf = ring("tmpf", 3, [128, 512], F32, ps_)
            mT = ring("mT", 2, [128, 8, 512], BF16, ps_)
            xr = ring("xr2", 3, [128, D], F32, ps_)
            xo = ring("xo2", 3, [128, D], F32, ps_)
            tmx = ring("tmx", 2, [128, 512], F32, ps_)
            pz = Ring(PB[0:3])
            po = Ring(PB[3:7])
            blocks = [(b * 512, 512) for b in range(4 if last else 8)] + ([(S, C)] if upd_ctx else [])
            if last:
                yb2r = ring("yb2r", 1, [128, 16, 512], BF16, ps_)
                sg2r = ring("sg2r", 2, [128, 4, 512], BF16, ps_)
                x2r = ring("x2r", 2, [128, D], F32, ps_)
            H = S // 2
            def merge_part(t0, nt):
                i = 0 if t0 < S else 1
                yb = ybr.next()
                dma("sp", yb[:, :, 0:nt], Y[:, t0:t0 + nt].rearrange("(j p) t -> p j t", p=128), r=[Y], w=[yb])
                if last:
                    yb2 = yb2r.next()
                    dma("sp", yb2[:, :, 0:nt], Y[:, t0 + H:t0 + H + nt].rearrange("(j p) t -> p j t", p=128), r=[Y], w=[yb2])
                    blend(yb, yb[:, :, 0:nt], yb2, yb2[:, :, 0:nt])
                m_ = mT.next()
                for dc in range(8):
                    sg_ = sgr.next()
                    dma("sp", sg_[:, :, 0:nt],
                        SG[:, t0:t0 + nt].rearrange("(n j p) t -> p n j t", p=128, j=8)[:, :, dc, :], r=[SG], w=[sg_])
                    if last:
                        sg2 = sg2r.next()
                        dma("sp", sg2[:, :, 0:nt],
                            SG[:, t0 + H:t0 + H + nt].rearrange("(n j p) t -> p n j t", p=128, j=8)[:, :, dc, :], r=[SG], w=[sg2])
                        blend(sg_, sg_[:, :, 0:nt], sg2, sg2[:, :, 0:nt])
                    af = accf.next()
                    for n in range(4):
                        ps = pz.next()
                        for kc in range(4):
                            op("pe", lambda e: e.matmul(ps[:, 0:nt], lhsT=wbr[:, n * 4 + kc, dc * 128:(dc + 1) * 128],
                                                        rhs=yb[:, n * 4 + kc, 0:nt], start=(kc == 0), stop=(kc == 3)),
                               r=[wbr, yb], w=[ps])
                        if n == 0:
                            op("dve", lambda e: e.tensor_tensor(out=af[:, 0:nt], in0=ps[:, 0:nt], in1=sg_[:, n, 0:nt],
                                                                op=ALU.mult), r=[ps, sg_], w=[af])
                        else:
                            tf = tmpf.next()
                            op("dve", lambda e: e.tensor_tensor(out=tf[:, 0:nt], in0=ps[:, 0:nt], in1=sg_[:, n, 0:nt],
                                                                op=ALU.mult), r=[ps, sg_], w=[tf])
                            if n < 3:
                                op("pool", lambda e: e.tensor_tensor(out=af[:, 0:nt], in0=af[:, 0:nt], in1=tf[:, 0:nt], op=ALU.add),
                                   r=[af, tf], w=[af])
                            else:
                                op("pool", lambda e: e.tensor_tensor(out=m_[:, dc, 0:nt], in0=af[:, 0:nt], in1=tf[:, 0:nt],
                                                                     op=ALU.add), r=[af, tf], w=[m_])
                return (t0, nt, i, m_)

            def out_part(ctx_):
                t0, nt, i, m_ = ctx_
                for tt in range(nt // 128):
                    t = t0 // 128 + tt
                    xt = xr.next()
                    src, srct = xsrc(l, t)
                    dma("sp", xt[:], src, r=[srct], w=[xt])
                    if last:
                        xt2 = x2r.next()
                        dma("sp", xt2[:], X[(t + 16) * 128:(t + 17) * 128, :], r=[X], w=[xt2])
                        blend(xt, xt[:], xt2, xt2[:])
                    xn_ = xo.next()
                    for hf in range(2):
                        ps = po.next()
                        for dc in range(8):
                            op("pe", lambda e: e.matmul(ps[:], lhsT=m_[:, dc, tt * 128:(tt + 1) * 128],
                                                        rhs=wo[:, dc, hf * 512:(hf + 1) * 512], start=(dc == 0), stop=(dc == 7)),
                               r=[m_, wo], w=[ps])
                        tx = tmx.next()
                        op("dve", lambda e: e.tensor_tensor(out=tx[:], in0=ps[:], in1=gbc[0][i][:, hf * 512:(hf + 1) * 512],
                                                            op=ALU.mult), r=[ps, gbc[0][i]], w=[tx])
                        op("pool", lambda e: e.tensor_tensor(out=xn_[:, hf * 512:(hf + 1) * 512], in0=tx[:],
                                                             in1=xt[:, hf * 512:(hf + 1) * 512], op=ALU.add), r=[tx, xt], w=[xn_])
                    if last:
                        dma("sp", X2[t * 128:(t + 1) * 128, :], xn_[:], r=[xn_], w=[X2])
                    else:
                        dma("sp", X[t * 128:(t + 1) * 128, :], xn_[:], r=[xn_], w=[X])
            ctxs = []
            for bi in range(len(blocks) + 1):
                if bi < len(blocks):
                    ctxs.append(merge_part(*blocks[bi]))
                if bi >= 1:
                    out_part(ctxs[bi - 1])
        if done(l, "R"):
            break

        sc.barrier()
        with contextlib.ExitStack() as ps_:
            groups = [list(range(0, 12)), list(range(12, 24)), list(range(24, ntl))] if not last else [list(range(0, 8)), list(range(8, 16))]
            XE = X2 if last else X
            maxg = max(len(g_) for g_ in groups)
            PT = alloc_pt(ps_)
            h2T = sb("h2T", [128, 8, maxg * 128], BF16, ps_)
            h2Ts = [Tile(h2T.ap, "h2T%d" % b_) for b_ in range(3)]
            gate = sb("gate", [128, maxg, NE], F32, ps_)
            acc = sb("accm", [128, maxg, D], F32, ps_)
            xr = ring("xr3", 2, [128, D], F32, ps_)
            xnr = ring("xnr3", 3, [128, D], BF16, ps_)
            ssr = ring("ssr3", 3, [128, 4], F32, ps_)
            wgu = ring("wgu", 2, [128, 8, D], BF16, ps_)
            wdn = ring("wdn", 2, [128, 4, D], BF16, ps_)
            silr = ring("silr", 2, [128, 512], F32, ps_)
            hmr = ring("hmr", 2, [128, 4, 512], BF16, ps_)
            xo = ring("xo3", 2, [128, D], F32, ps_)
            gS = sb("gS", [128, maxg, NE], F32, ps_)
            gSel = sb("gSel", [128, maxg, NE], F32, ps_)
            gA = sb("gA", [128, maxg, NE], F32, ps_)
            gB = sb("gB", [128, maxg, NE], F32, ps_)
            gM = [sb("gM%d" % i_, [128, maxg * 4], F32, ps_) for i_ in range(4)]
            gT = sb("gT", [128, 4, maxg], F32, ps_)
            pgu = Ring(PB[0:4])
            pdn = Ring(PB[4:7])
            for grp in groups:
                ng = len(grp)
                for ti, t in enumerate(grp):
                    i = 0 if t < 32 else 1
                    xt = xr.next()
                    dma("sp", xt[:], XE[t * 128:(t + 1) * 128, :], r=[XE], w=[xt])
                    norm_tile(PT, xt, h2T, ti * 128, sc2, 24, i, xnr, ssr, trk=h2Ts[ti // 4])
                    pr_ = PB[5 + (ti % 2)]
                    for k in range(8):
                        op("pe", lambda e: e.matmul(pr_[:, 0:NE], lhsT=h2T[:, k, ti * 128:(ti + 1) * 128], rhs=wr[:, k, :],
                                                    start=(k == 0), stop=(k == 7)), r=[h2Ts[ti // 4], wr], w=[pr_])
                    op("act", lambda e: e.activation(out=gS[:, ti, :], in_=pr_[:, 0:NE], func=AF.Sigmoid), r=[pr_], w=[gS])
                G4 = ng * 4
                v16 = lambda t_: t_[:, 0:ng, :]
                v44 = lambda t_: t_[:, 0:ng, :].rearrange("p g (a e) -> p (g a) e", e=4)
                flat = lambda t_: t_[:, 0:ng, :].rearrange("p g e -> p (g e)")
                m1, m2, gs, pen = gM
                op("dve", lambda e: e.tensor_tensor(out=v16(gSel), in0=v16(gS), in1=rbt[:, 0:ng, :], op=ALU.add), r=[gS, rbt], w=[gSel])
                op("dve", lambda e: e.tensor_reduce(out=m1[:, 0:G4], in_=v44(gSel), axis=AX.X, op=ALU.max), r=[gSel], w=[m1])
                op("dve", lambda e: e.tensor_tensor(out=v44(gA), in0=v44(gSel), in1=m1[:, 0:G4].unsqueeze(2).broadcast_to([128, G4, 4]),
                                                    op=ALU.is_equal), r=[gSel, m1], w=[gA])
                op("dve", lambda e: e.scalar_tensor_tensor(out=flat(gB), in0=flat(gA), scalar=-BIG, in1=flat(gSel), op0=ALU.mult,
                                                           op1=ALU.add), r=[gA, gSel], w=[gB])
                op("dve", lambda e: e.tensor_reduce(out=m2[:, 0:G4], in_=v44(gB), axis=AX.X, op=ALU.max), r=[gB], w=[m2])
                op("dve", lambda e: e.tensor_tensor(out=gs[:, 0:G4], in0=m1[:, 0:G4], in1=m2[:, 0:G4], op=ALU.add), r=[m1, m2], w=[gs])
                op("dve", lambda e: e.tensor_reduce(out=gT[:, 0, 0:ng], in_=gs[:, 0:G4].rearrange("p (g a) -> p g a", a=4), axis=AX.X,
                                                    op=ALU.max), r=[gs], w=[gT])
                op("dve", lambda e: e.tensor_tensor(out=pen[:, 0:G4].rearrange("p (g a) -> p g a", a=4),
                                                    in0=gs[:, 0:G4].rearrange("p (g a) -> p g a", a=4),
                                                    in1=gT[:, 0, 0:ng].unsqueeze(2).broadcast_to([128, ng, 4]), op=ALU.is_equal),
                   r=[gs, gT], w=[pen])
                op("dve", lambda e: e.tensor_scalar(out=pen[:, 0:G4], in0=pen[:, 0:G4], scalar1=-1.0, scalar2=BIG, op0=ALU.add,
                                                    op1=ALU.mult), r=[pen], w=[pen])
                op("dve", lambda e: e.tensor_tensor(out=v44(gA), in0=v44(gSel), in1=pen[:, 0:G4].unsqueeze(2).broadcast_to([128, G4, 4]),
                                                    op=ALU.add), r=[gSel, pen], w=[gA])
                op("dve", lambda e: e.tensor_reduce(out=gT[:, 1, 0:ng], in_=v16(gA), axis=AX.X, op=ALU.max), r=[gA], w=[gT])
                op("dve", lambda e: e.tensor_tensor(out=v16(gB), in0=v16(gA), in1=gT[:, 1, 0:ng].unsqueeze(2).broadcast_to([128, ng, NE]),
                                                    op=ALU.is_equal), r=[gA, gT], w=[gB])
                op("dve", lambda e: e.scalar_tensor_tensor(out=flat(gSel), in0=flat(gB), scalar=-BIG, in1=flat(gA), op0=ALU.mult,
                                                           op1=ALU.add), r=[gB, gA], w=[gSel])
                op("dve", lambda e: e.tensor_reduce(out=gT[:, 2, 0:ng], in_=v16(gSel), axis=AX.X, op=ALU.max), r=[gSel], w=[gT])
                op("dve", lambda e: e.tensor_tensor(out=v16(gA), in0=v16(gSel), in1=gT[:, 2, 0:ng].unsqueeze(2).broadcast_to([128, ng, NE]),
                                                    op=ALU.is_equal), r=[gSel, gT], w=[gA])
                op("dve", lambda e: e.tensor_tensor(out=flat(gA), in0=flat(gA), in1=flat(gB), op=ALU.add), r=[gA, gB], w=[gA])
                op("dve", lambda e: e.tensor_tensor(out=flat(gB), in0=flat(gA), in1=flat(gS), op=ALU.mult), r=[gA, gS], w=[gB])
                op("dve", lambda e: e.tensor_reduce(out=gT[:, 3, 0:ng], in_=v16(gB), axis=AX.X, op=ALU.add), r=[gB], w=[gT])
                op("dve", lambda e: e.reciprocal(out=gT[:, 3, 0:ng], in_=gT[:, 3, 0:ng]), r=[gT], w=[gT])
                op("dve", lambda e: e.tensor_tensor(out=gate[:, 0:ng, :], in0=v16(gB),
                                                    in1=gT[:, 3, 0:ng].unsqueeze(2).broadcast_to([128, ng, NE]), op=ALU.mult),
                   r=[gB, gT], w=[gate])
                nblk = (ng + 3) // 4
                def gu_part(ex, b, wts):
                    if b == 0:
                        wg_ = wgu.next()
                        wd_ = wdn.next()
                        dma("pool", wg_[:], w_gu[l, ex].rearrange("(k p) n -> p k n", p=128), w=[wg_])
                        dma("pool", wd_[:], w_dn[l, ex].rearrange("(k p) n -> p k n", p=128), w=[wd_])
                        wts[ex] = (wg_, wd_)
                    wg_, wd_ = wts[ex]
                    tl0 = b * 4
                    ntile = min(4, ng - tl0)
                    nt = ntile * 128
                    c0 = tl0 * 128
                    hm = hmr.next()
                    for cch in range(4):
                        pa_ = pgu.next()
                        pb_ = pgu.next()
                        for k in range(8):
                            op("pe", lambda e: e.matmul(pa_[:, 0:nt], lhsT=wg_[:, k, cch * 128:(cch + 1) * 128],
                                                        rhs=h2T[:, k, c0:c0 + nt], start=(k == 0), stop=(k == 7)),
                               r=[wg_, h2Ts[b]], w=[pa_])
                        for k in range(8):
                            op("pe", lambda e: e.matmul(pb_[:, 0:nt], lhsT=wg_[:, k, 512 + cch * 128:512 + (cch + 1) * 128],
                                                        rhs=h2T[:, k, c0:c0 + nt], start=(k == 0), stop=(k == 7)),
                               r=[wg_, h2Ts[b]], w=[pb_])
                        sl = silr.next()
                        op("act", lambda e: e.activation(out=sl[:, 0:nt], in_=pa_[:, 0:nt], func=AF.Silu), r=[pa_], w=[sl])
                        op("dve", lambda e: e.tensor_tensor(out=hm[:, cch, 0:nt], in0=pb_[:, 0:nt], in1=sl[:, 0:nt],
                                                            op=ALU.mult), r=[pb_, sl], w=[hm])
                    return (ex, tl0, ntile, hm, wd_)

                def dn_part(ctx_):
                    ex, tl0, ntile, hm, wd_ = ctx_
                    for tt in range(ntile):
                        ti = tl0 + tt
                        for hf in range(2):
                            pd = pdn.next()
                            for cch in range(4):
                                op("pe", lambda e: e.matmul(pd[:], lhsT=hm[:, cch, tt * 128:(tt + 1) * 128],
                                                            rhs=wd_[:, cch, hf * 512:(hf + 1) * 512], start=(cch == 0),
                                                            stop=(cch == 3)), r=[hm, wd_], w=[pd])
                            if ex == 0:
                                op("dve", lambda e: e.tensor_scalar(out=acc[:, ti, hf * 512:(hf + 1) * 512], in0=pd[:],
                                                                    scalar1=gate[:, ti, ex:ex + 1], scalar2=None,
                                                                    op0=ALU.mult), r=[pd, gate], w=[acc])
                            else:
                                op("dve", lambda e: e.scalar_tensor_tensor(out=acc[:, ti, hf * 512:(hf + 1) * 512], in0=pd[:],
                                                                           scalar=gate[:, ti, ex:ex + 1],
                                                                           in1=acc[:, ti, hf * 512:(hf + 1) * 512],
                                                                           op0=ALU.mult, op1=ALU.add),
                                   r=[pd, gate, acc], w=[acc])
                wts = {}
                pend = None
                for ex in range(NE):
                    for b in range(nblk):
                        cur_ = gu_part(ex, b, wts)
                        if pend is not None:
                            dn_part(pend)
                        pend = cur_
                dn_part(pend)
                for ti, t in enumerate(grp):
                    i = 0 if t < 32 else 1
                    xt = xr.next()
                    dma("sp", xt[:], XE[t * 128:(t + 1) * 128, :], r=[XE], w=[xt])
                    xn_ = xo.next()
                    op("dve", lambda e: e.tensor_tensor(out=acc[:, ti, :], in0=acc[:, ti, :], in1=gbc[1][i][:], op=ALU.mult),
                       r=[acc, gbc[1][i]], w=[acc])
                    op("dve", lambda e: e.tensor_tensor(out=xn_[:], in0=acc[:, ti, :], in1=xt[:], op=ALU.add), r=[acc, xt], w=[xn_])
                    if not last:
                        dma("sp", X[t * 128:(t + 1) * 128, :], xn_[:], r=[xn_], w=[X])
                    else:
                        junk = xnr.next()
                        ss = ssr.next()
                        op("act", lambda e: e.activation(out=junk[:], in_=xn_[:], func=AF.Square, accum_out=ss[:, 0:1]),
                           r=[xn_], w=[junk, ss])
                        op("act", lambda e: e.activation(out=ss[:, 1:2], in_=ss[:, 0:1], func=AF.Sqrt, scale=1.0 / D,
                                                         bias=epsc[:, 0:1]), r=[ss, epsc], w=[ss])
                        op("dve", lambda e: e.reciprocal(out=ss[:, 2:3], in_=ss[:, 1:2]), r=[ss], w=[ss])
                        xf = xr.next()
                        op("dve", lambda e: e.scalar_tensor_tensor(out=xf[:], in0=xn_[:], scalar=ss[:, 2:3], in1=fngt[:],
                                                                   op0=ALU.mult, op1=ALU.mult), r=[xn_, ss, fngt], w=[xf])
                        dma("sp", out_d[t * 128:(t + 1) * 128, :], xf[:], r=[xf], w=[X])
        if done(l, "E"):
            break

    sc.barrier()


def _const_tables():
    p = np.arange(128)
    ident = np.eye(128, dtype=np.float32)
    ones = np.ones((128, 128), np.float32)
    bd = np.kron(np.eye(2, dtype=np.float32), np.ones((64, 64), np.float32))
    Pm = np.zeros((128, 128), np.float32)
    for i in range(64):
        Pm[2 * i + 1, 2 * i] = -1.0
        Pm[2 * i, 2 * i + 1] = 1.0
    ang = 2.0 * np.pi * np.outer(p, p) / 128.0
    CC = np.cos(ang).astype(np.float32)
    SC = np.sin(ang).astype(np.float32)
    c128 = np.concatenate([ident, ones, bd, Pm, CC, SC], axis=1)
    t = np.arange(S)
    row = (t // 64).astype(np.float32)
    col = (t % 64).astype(np.float32)
    inv = (10000.0 ** (-np.arange(16, dtype=np.float32) / 16)).astype(np.float32)
    angt = np.concatenate([row[:, None] * inv, col[:, None] * inv], axis=-1).astype(np.float32)
    j = (p % 64) // 2
    rope = np.stack([np.cos(angt)[:, j].T, np.sin(angt)[:, j].T], axis=1).astype(np.float32)
    kc = p % 64
    half = p // 64
    qc = np.arange(64)
    c0 = np.clip(qc - 8, 0, 48)
    colm = ((kc[:, None] >= c0[None, :]) & (kc[:, None] < c0[None, :] + 16)).astype(np.float32)
    dr = np.arange(16) - 8
    drv = ((dr[None, :] + half[:, None] >= -7) & (dr[None, :] + half[:, None] <= 7)).astype(np.float32)
    namask = (drv[:, :, None] * colm[:, None, :]).reshape(128, 16 * 64).astype(np.float32)
    n = np.arange(256)
    a256 = 2.0 * np.pi * np.outer(n, n) / 256.0
    d256 = np.stack([np.cos(a256), -np.sin(a256)], 0).reshape(2, 2, 128, 256).transpose(2, 0, 1, 3).astype(np.float32)
    n1 = np.arange(64)
    a64 = 2.0 * np.pi * np.outer(n1, n1) / 64.0
    f1 = np.concatenate([np.cos(a64), -np.sin(a64)], axis=1).astype(np.float32)
    k1 = np.arange(64)
    k2 = np.arange(64)
    kk = k1[:, None] + 64 * k2[None, :]
    am = 2.0 * np.pi * n1[:, None, None] * kk[None, :, :] / 4096.0
    Mre = np.cos(am)
    Mim = -np.sin(am)
    M1 = np.concatenate([Mre, Mim], axis=2)
    M2 = np.concatenate([-Mim, Mre], axis=2)
    m12 = np.stack([M1, M2], axis=1).reshape(64, 2, 64 * 128).astype(np.float32)
    return dict(c128=c128, rope=rope, namask=namask, dft256=np.ascontiguousarray(d256), f1=f1, m12=m12)


def _col(v, k):
    return np.ascontiguousarray(v.reshape(k, 128).T)


def _prep_inputs(inp):
    f = lambda a: np.ascontiguousarray(np.asarray(a, dtype=np.float32))
    shared = dict(_const_tables())
    shared["w_mod"] = f(inp["w_mod"])
    shared["bmodT"] = np.stack([_col(f(inp["b_mod"])[l], 48) for l in range(DEPTH)])
    shared["n1g"] = np.stack([_col(f(inp["norm1_g"])[l], 8) for l in range(DEPTH)])
    shared["n2g"] = np.stack([_col(f(inp["norm2_g"])[l], 8) for l in range(DEPTH)])
    shared["w_in"] = f(inp["w_in"])
    shared["qgc"] = np.stack([np.tile(f(inp["q_norm_g"])[l], 2)[:, None] for l in range(DEPTH)])
    shared["kgc"] = np.stack([np.tile(f(inp["k_norm_g"])[l], 2)[:, None] for l in range(DEPTH)])
    rpb = f(inp["na_rpb"])
    rp = np.zeros((DEPTH, 8, 17, 127), np.float32)
    rp[:, :, 1:16, 48:79] = rpb[:, :, :, ::-1]
    shared["rpbP"] = rp
    cw = f(inp["conv_w"])
    shared["cwT"] = np.ascontiguousarray(cw.reshape(DEPTH, 31, 4, 128).transpose(0, 3, 2, 1))
    shared["cbT"] = np.stack([_col(f(inp["conv_b"])[l], 4) for l in range(DEPTH)])
    shared["cgT"] = np.stack([_col(f(inp["conv_norm_g"])[l], 4) for l in range(DEPTH)])
    shared["w_branch"] = f(inp["w_branch"])
    shared["w_out"] = f(inp["w_out"])
    shared["wr"] = np.ascontiguousarray(f(inp["w_router"]).reshape(8, 128, NE).transpose(1, 0, 2))
    shared["rb"] = np.ascontiguousarray(np.broadcast_to(f(inp["router_bias"])[None, None, :], (128, 12, NE)))
    shared["w_gu"] = f(inp["w_expert_gu"])
    shared["w_dn"] = f(inp["w_expert_down"])
    shared["fng"] = np.ascontiguousarray(np.broadcast_to(f(inp["final_norm_g"])[None, :], (128, D)))
    x = f(inp["x"])
    c = f(inp["c"])
    ctx = f(inp["ctx"])
    cc = f(inp["c_ctx"])
    maps = []
    for core in range(8):
        b = core % 4
        m = dict(shared)
        m["x"] = x[b]
        m["ctx"] = ctx[b]
        m["cvec"] = np.ascontiguousarray(np.stack([_col(c[b], 8), _col(cc, 8)], axis=-1))
        half = core // 4
        m["sel"] = np.ascontiguousarray(np.broadcast_to(np.array([1.0 - half, float(half)], np.float32)[None, :], (128, 2)))
        maps.append(m)
    return maps


_NC_CACHE = {}


def kernel(**inputs):
    maps = _prep_inputs(inputs)
    if "nc" not in _NC_CACHE:
        _NC_CACHE["nc"] = build()
    nc = _NC_CACHE["nc"]
    res = run_bass_kernel_spmd(nc, maps, core_ids=list(range(8)))
    out = np.empty((4, S, D), np.float32)
    for core in range(8):
        b, half = core % 4, core // 4
        out[b, half * (S // 2):(half + 1) * (S // 2)] = np.asarray(res.results[core]["out"], dtype=np.float32)
    return out
```
